# Optimizing a Trainium2 kernel written in Bass

```python
import math
import jax, jax.numpy as jnp
from jax import lax
import numpy as np

D_MODEL = 1024
BATCH = 16
SEQ = 4096
DEPTH = 2

CHUNK = 64
N_MIXERS = 2
MIX_WIDTH = D_MODEL // 2
SGU_CHUNK = 128
SGU_HEADS = 4
SGU_HEAD_DIM = MIX_WIDTH // SGU_HEADS
CONV_WIDTH = 3
N_MEM = 256
XA_HEADS = 4
XA_HEAD_DIM = MIX_WIDTH // XA_HEADS
N_EXPERTS = 16
N_GROUPS = 4
GROUP_SIZE = N_EXPERTS // N_GROUPS
TOP_K = 2
D_EXPERT = D_MODEL // 2
N_A = (DEPTH + 1) // 2
N_B = DEPTH // 2
ALPHA = (2 * DEPTH) ** 0.25
BETA = (8 * DEPTH) ** -0.25
LN_EPS = 1e-5

kernel_name = "hybrid_gmlp_shortconv_memxattn_grouped_moe"


def layer_norm(x, g, b):
    xf = x.astype(jnp.float32)
    mu = xf.mean(-1, keepdims=True)
    var = jnp.square(xf - mu).mean(-1, keepdims=True)
    y = (xf - mu) * lax.rsqrt(var + LN_EPS) * g.astype(jnp.float32) + b.astype(jnp.float32)
    return y.astype(x.dtype)


def spatial_gating(z, ln_g, ln_b, w_s, b_s):
    u, v = jnp.split(z, 2, axis=-1)
    v = layer_norm(v, ln_g, ln_b)
    bsz, seq, _ = v.shape
    v = v.reshape(bsz, seq // SGU_CHUNK, SGU_CHUNK, SGU_HEADS, SGU_HEAD_DIM)
    frame_chunk = jnp.arange(SGU_CHUNK) // CHUNK
    mask = frame_chunk[:, None] >= frame_chunk[None, :]
    w = jnp.where(mask[None], w_s, jnp.zeros_like(w_s))
    mixed = jnp.einsum('hij,bcjhd->bcihd', w, v) + b_s.T[None, None, :, :, None]
    return u * mixed.reshape(bsz, seq, MIX_WIDTH)


def short_conv_mixer(z, conv_w):
    bg, cg, xt = jnp.split(z, 3, axis=-1)
    h = cg * xt
    h = lax.conv_general_dilated(
        h, conv_w[:, None, :].astype(h.dtype), window_strides=(1,),
        padding=[(CONV_WIDTH - 1, 0)], dimension_numbers=('NWC', 'WIO', 'NWC'),
        feature_group_count=MIX_WIDTH)
    return bg * h


def memory_cross_attention(q, mem, w_kv):
    bsz, seq, _ = q.shape
    n_mem = mem.shape[1]
    k, v = jnp.split(mem @ w_kv, 2, axis=-1)
    q = q.reshape(bsz, seq, XA_HEADS, XA_HEAD_DIM)
    k = k.reshape(bsz, n_mem, XA_HEADS, XA_HEAD_DIM)
    v = v.reshape(bsz, n_mem, XA_HEADS, XA_HEAD_DIM)
    s = jnp.einsum('bshd,bmhd->bhsm', q, k).astype(jnp.float32) * (XA_HEAD_DIM ** -0.5)
    p = jax.nn.softmax(s, axis=-1).astype(v.dtype)
    o = jnp.einsum('bhsm,bmhd->bshd', p, v)
    return o.reshape(bsz, seq, MIX_WIDTH)


def grouped_moe(x, router_w, router_b, w_gate, w_up, w_down):
    bsz, seq, d = x.shape
    xt = x.reshape(-1, d)
    scores = jax.nn.sigmoid(jnp.dot(xt.astype(jnp.float32), router_w.astype(jnp.float32)))
    sel = scores + router_b.astype(jnp.float32)
    group_score = lax.top_k(sel.reshape(-1, N_GROUPS, GROUP_SIZE), TOP_K)[0].sum(-1)
    top_group = jnp.argmax(group_score, axis=-1)
    expert_group = jnp.arange(N_EXPERTS) // GROUP_SIZE
    in_group = expert_group[None, :] == top_group[:, None]
    _, idx = lax.top_k(jnp.where(in_group, sel, -jnp.inf), TOP_K)
    gate = jnp.take_along_axis(scores, idx, axis=-1)
    gate = gate / gate.sum(-1, keepdims=True)
    combine = jnp.einsum('tk,tke->te', gate,
                         jax.nn.one_hot(idx, N_EXPERTS, dtype=jnp.float32)).astype(x.dtype)
    out = jnp.zeros_like(xt)
    for e in range(N_EXPERTS):
        h = jax.nn.silu(xt @ w_gate[e]) * (xt @ w_up[e])
        out = out + combine[:, e:e + 1] * (h @ w_down[e])
    return out.reshape(bsz, seq, d)


def setup_inputs(seed: int = 0) -> dict:
    key = jax.random.key(seed)
    ks = jax.random.split(key, 20)
    d = D_MODEL

    def nrm(k, shape, scale):
        return jax.random.normal(k, shape, jnp.float32) * scale

    x = nrm(ks[0], (BATCH, SEQ, d), 1.0)
    mem = nrm(ks[1], (BATCH, N_MEM, d), 1.0)
    w_in_a = nrm(ks[2], (N_A, d, 3 * MIX_WIDTH), d ** -0.5)
    sgu_ln_g = 1.0 + nrm(ks[3], (N_A, MIX_WIDTH), 0.02)
    sgu_ln_b = nrm(ks[4], (N_A, MIX_WIDTH), 0.02)
    sgu_w = nrm(ks[5], (N_A, SGU_HEADS, SGU_CHUNK, SGU_CHUNK), SGU_CHUNK ** -0.5)
    sgu_b = 1.0 + nrm(ks[6], (N_A, SGU_HEADS, SGU_CHUNK), 0.02)
    w_in_b = nrm(ks[7], (N_B, d, 4 * MIX_WIDTH), d ** -0.5)
    conv_w = nrm(ks[8], (N_B, CONV_WIDTH, MIX_WIDTH), CONV_WIDTH ** -0.5)
    kv_scale = jnp.concatenate([jnp.ones((MIX_WIDTH,), jnp.float32),
                                jnp.full((MIX_WIDTH,), BETA, jnp.float32)]) * (d ** -0.5)
    w_kv = nrm(ks[9], (DEPTH, d, 2 * MIX_WIDTH), 1.0) * kv_scale
    w_out = nrm(ks[10], (DEPTH, 2 * MIX_WIDTH, d), BETA * (2 * MIX_WIDTH) ** -0.5)
    ln1_g = 1.0 + nrm(ks[11], (DEPTH, d), 0.02)
    ln1_b = nrm(ks[12], (DEPTH, d), 0.02)
    router_w = nrm(ks[13], (d, N_EXPERTS), d ** -0.5)
    router_b = nrm(ks[14], (N_EXPERTS,), 0.01)
    w_gate = nrm(ks[15], (DEPTH, N_EXPERTS, d, D_EXPERT), d ** -0.5)
    w_up = nrm(ks[16], (DEPTH, N_EXPERTS, d, D_EXPERT), d ** -0.5)
    w_down = nrm(ks[17], (DEPTH, N_EXPERTS, D_EXPERT, d), BETA * D_EXPERT ** -0.5)
    ln2_g = 1.0 + nrm(ks[18], (DEPTH, d), 0.02)
    ln2_b = nrm(ks[19], (DEPTH, d), 0.02)
    return {"x": x, "mem": mem, "w_in_a": w_in_a, "sgu_ln_g": sgu_ln_g, "sgu_ln_b": sgu_ln_b,
            "sgu_w": sgu_w, "sgu_b": sgu_b, "w_in_b": w_in_b, "conv_w": conv_w,
            "w_kv": w_kv, "w_out": w_out, "ln1_g": ln1_g, "ln1_b": ln1_b,
            "router_w": router_w, "router_b": router_b, "w_gate": w_gate, "w_up": w_up,
            "w_down": w_down, "ln2_g": ln2_g, "ln2_b": ln2_b}


def reference(x, mem, w_in_a, sgu_ln_g, sgu_ln_b, sgu_w, sgu_b, w_in_b, conv_w,
              w_kv, w_out, ln1_g, ln1_b, router_w, router_b, w_gate, w_up, w_down,
              ln2_g, ln2_b):
    for i in range(DEPTH):
        j = i // N_MIXERS
        if i % N_MIXERS == 0:
            z = x @ w_in_a[j]
            uv = jax.nn.gelu(z[..., :2 * MIX_WIDTH], approximate=False)
            tok = spatial_gating(uv, sgu_ln_g[j], sgu_ln_b[j], sgu_w[j], sgu_b[j])
            q = z[..., 2 * MIX_WIDTH:]
        else:
            z = x @ w_in_b[j]
            tok = short_conv_mixer(z[..., :3 * MIX_WIDTH], conv_w[j])
            q = z[..., 3 * MIX_WIDTH:]
        xa = memory_cross_attention(q, mem, w_kv[i])
        o = jnp.concatenate([tok, xa], axis=-1) @ w_out[i]
        x = layer_norm(ALPHA * x + o, ln1_g[i], ln1_b[i])
        f = grouped_moe(x, router_w, router_b, w_gate[i], w_up[i], w_down[i])
        x = layer_norm(ALPHA * x + f, ln2_g[i], ln2_b[i])
    return x
```

```python
import numpy as np
import concourse.bass as bass
import concourse.mybir as mybir
from concourse.bass_utils import run_bass_kernel_spmd

F32 = mybir.dt.float32
BF16 = mybir.dt.bfloat16
I32 = mybir.dt.int32
AF = mybir.ActivationFunctionType
ALU = mybir.AluOpType
AX = mybir.AxisListType

D = 1024
MIXW = 512
N_MEM = 256
NE = 16
DEPTH = 2
ALPHA = float((2 * DEPTH) ** 0.25)
EPS = 1e-5
SCALE = float(128 ** -0.5)
TT = 1024
NCH = TT // 128
NBLK = TT // 512
NSLOT = 7
NCAST = 8


class Op:
    __slots__ = ("eng", "fn", "dma", "deps", "needed", "sig")

    def __init__(self, eng, fn, dma):
        self.eng = eng
        self.fn = fn
        self.dma = dma
        self.deps = ()
        self.needed = False
        self.sig = None


class Prog:
    ENG = ("pe", "act", "dve", "pool", "sp")

    def __init__(self):
        self.ops = {e: [] for e in self.ENG}
        self.lastw = {}
        self.readers = {}
        self.dma_last = {}
        self.final = []

    def add(self, eng, fn, reads=(), writes=(), dma=None):
        op = Op(eng, fn, dma)
        deps = {}
        wset = set(writes)
        for r in reads:
            if isinstance(r, tuple) and r[0] == "PB":
                wset.add(r)
        for r in reads:
            w = self.lastw.get(r)
            if w is not None:
                deps[id(w)] = w
        for r in wset:
            w = self.lastw.get(r)
            if w is not None:
                deps[id(w)] = w
            rd = self.readers.get(r)
            if rd:
                for o in rd.values():
                    deps[id(o)] = o
        if dma is not None:
            prev = self.dma_last.get(dma)
            if prev is not None:
                deps[id(prev)] = prev
            self.dma_last[dma] = op
        op.deps = [d for d in deps.values()
                   if not (d.eng == "pe" and eng == "pe" and d.dma is None and dma is None)]
        for r in wset:
            self.lastw[r] = op
            self.readers[r] = {}
        for r in reads:
            if r not in wset:
                self.readers.setdefault(r, {})[eng] = op
        self.ops[eng].append(op)
        return op

    def emit(self, nc, engsem, dmasem):
        for e in self.ENG:
            for op in self.ops[e]:
                for d in op.deps:
                    d.needed = True
        dcnt = {}
        for e in self.ENG:
            cnt = 0
            for op in self.ops[e]:
                if op.dma is not None:
                    dcnt[op.dma] = dcnt.get(op.dma, 0) + 16
                    op.sig = (("dma", op.dma), dcnt[op.dma])
                elif op.needed:
                    cnt += 1
                    op.sig = (("eng", e), cnt)
        finals = [(("dma", k), v) for k, v in dcnt.items()]

        def handle(key):
            return dmasem[key[1]] if key[0] == "dma" else engsem[key[1]]

        def run(e, eng):
            seen = {}
            for op in self.ops[e]:
                for d in op.deps:
                    key, val = d.sig
                    if seen.get(key, 0) >= val:
                        continue
                    eng.wait_ge(handle(key), val)
                    seen[key] = val
                ins = op.fn(eng)
                if op.dma is not None:
                    ins.then_inc(dmasem[op.dma], 16)
                elif op.needed:
                    ins.then_inc(engsem[e], 1)
            if e == "sp":
                for key, val in finals:
                    if seen.get(key, 0) < val:
                        eng.wait_ge(handle(key), val)

        with nc.Block() as block:
            @block.tensor
            def _(eng):
                run("pe", eng)

            @block.scalar
            def _(eng):
                run("act", eng)

            @block.vector
            def _(eng):
                run("dve", eng)

            @block.gpsimd
            def _(eng):
                run("pool", eng)

            @block.sync
            def _(eng):
                run("sp", eng)


def build(n_seq, seq_len, layers, dbg=None):
    assert seq_len % TT == 0
    tiles_per_seq = seq_len // TT
    ntok = n_seq * seq_len
    nc = bass.Bass("TRN2", target_bir_lowering=False)

    def din(name, shape):
        return nc.dram_tensor(name, list(shape), F32, kind="ExternalInput")

    x_t = din("x", [ntok, D])
    mem_t = din("mem", [n_seq * N_MEM, D])
    w_in_a_t = din("w_in_a", [1, D, 3 * MIXW])
    sgu_ln_g_t = din("sgu_ln_g", [1, MIXW])
    sgu_ln_b_t = din("sgu_ln_b", [1, MIXW])
    sgu_w_t = din("sgu_w", [1, 4, 128, 128])
    sgu_b_t = din("sgu_b", [1, 4 * 128])
    w_in_b_t = din("w_in_b", [1, D, 4 * MIXW])
    conv_w_t = din("conv_w", [1, 3, MIXW])
    w_kv_t = din("w_kv", [2, D, 2 * MIXW])
    w_out_t = din("w_out", [2, D, D])
    ln1_g_t = din("ln1_g", [2, D])
    ln1_b_t = din("ln1_b", [2, D])
    router_w_t = din("router_w", [D, NE])
    router_b_t = din("router_b", [1, NE])
    w_gate_t = din("w_gate", [2, NE, D, MIXW])
    w_up_t = din("w_up", [2, NE, D, MIXW])
    w_down_t = din("w_down", [2, NE, MIXW, D])
    ln2_g_t = din("ln2_g", [2, D])
    ln2_b_t = din("ln2_b", [2, D])
    y_t = nc.dram_tensor("y", [ntok, D], F32, kind="ExternalOutput")

    x_d, mem_d, y_d = x_t.ap(), mem_t.ap(), y_t.ap()

    unit_src = {}
    unit_ids = {}

    def reg_unit(key, src_ap):
        unit_ids[key] = len(unit_ids)
        unit_src[key] = src_ap

    def kn(ap2d, j):
        return ap2d[:, j * 512:(j + 1) * 512].rearrange("(kc p) n -> p kc n", p=128)

    for l in (0, 1):
        for j in range(2):
            reg_unit(("kv", l, j), kn(w_kv_t.ap()[l], j))
    for l in layers:
        nin = 3 if l == 0 else 4
        wi = w_in_a_t.ap()[0] if l == 0 else w_in_b_t.ap()[0]
        for j in range(nin):
            reg_unit(("in", l, j), kn(wi, j))
        for j in range(2):
            reg_unit(("out", l, j), kn(w_out_t.ap()[l], j))
        for e in range(NE):
            reg_unit(("g", l, e), kn(w_gate_t.ap()[l, e], 0))
            reg_unit(("u", l, e), kn(w_up_t.ap()[l, e], 0))
            reg_unit(("d", l, e), w_down_t.ap()[l, e].rearrange("(mc p) n -> p mc n", p=128))
    NU = len(unit_ids)
    wbf_t = nc.dram_tensor("wbf", [NU, 128, 4096], BF16, kind="Internal")
    wbf = wbf_t.ap()

    P = Prog()
    import contextlib
    es = contextlib.ExitStack()

    def sb(name, shape, dt):
        return es.enter_context(nc.sbuf_tensor(name, list(shape), dt))

    with es:
        R = sb("R", [128, NCH, D], F32)
        XT = sb("XT", [128, 8, TT], BF16)
        RING = sb("RING", [128, NSLOT, 4096], BF16)
        HTb = [sb(f"hT{i}", [128, 4, 512], BF16) for i in range(2)]
        SG = [sb(f"SG{i}", [128, 512], F32) for i in range(2)]
        CATTs = [sb(f"CATT{i}", [128, 8, 512], BF16) for i in range(2)]
        Ub = [sb(f"U{i}", [128, 512], F32) for i in range(2)]
        Tt = sb("Tt", [128, 512], F32)
        CGs = Tt
        QT = sb("QT", [128, 4, 512], BF16)
        VN = sb("VN", [128, 4, 512], BF16)
        VG = [sb(f"VG{i}", [128, 512], F32) for i in range(2)]
        Yc = VG[1][:, :]
        HTw = VN[:, :, :].rearrange("p a b -> p (a b)").bitcast(F32)[:, 0:516]
        ETall = sb("ETall", [128, 2, 2, 512], BF16)
        MT = ETall[:, :, :, :].rearrange("p a b c -> p (a b c)").rearrange("p (k m) -> p k m", k=8)
        PVS = ETall[:, 1, :, :].rearrange("p b c -> p (b c)").bitcast(F32)
        PVS_KEYS = [("ET", 1, 0), ("ET", 1, 1)]
        RDEN = sb("RDEN", [128, 512], F32)
        DG = RDEN[:, :].rearrange("p (c t) -> p c t", c=4)
        RT4 = sb("RT4", [128, 4], F32)
        ONESB = sb("ONESB", [128, 128], BF16)
        Yb = [sb(f"Y{i}", [128, D], F32) for i in range(2)]
        X1Ts = [sb(f"X1T{i}", [128, 8, 128], F32) for i in range(2)]
        HS = sb("HS", [128, 4, 2], F32)
        KT = [sb(f"KT{l}", [128, 4, N_MEM], BF16) for l in range(2)]
        Vv = [sb(f"V{l}", [128, 2, 512], BF16) for l in range(2)]
        LNP = [sb(f"LNP{i}", [128, D], F32) for i in range(4)]
        G1T = [sb(f"G1T{l}", [128, 8], F32) for l in range(2)]
        B1T = [sb(f"B1T{l}", [128, 8], F32) for l in range(2)]
        NMRs = [sb(f"NMR{i}", [128, 1], F32) for i in range(2)]
        SGUG = sb("SGUG", [128, 512], F32)
        SGUB = sb("SGUB", [128, 512], F32)
        BSB = sb("BSB", [128, 512], F32)
        WST = sb("WST", [128, 4, 128], BF16)
        WSF = Yc[:, :].rearrange("p (h j) -> p h j", h=4)
        CWT = sb("CWT", [128, 3, 4], F32)
        RW = sb("RW", [128, 8, NE], F32)
        RB = sb("RB", [128, NE], F32)
        CW = sb("CW", [128, NCH, NE], F32)
        IDF = sb("IDF", [128, 128], F32)
        IOT = sb("IOT", [128, 128], I32)
        NEGH = sb("NEGH", [128, 1], F32)
        STs = [sb(f"ST{i}", [128, 2, 6], F32) for i in range(4)]
        MVs = [sb(f"MV{i}", [128, 2], F32) for i in range(4)]
        VEs = [sb(f"VE{i}", [128, 1], F32) for i in range(4)]
        RSTDs = [sb(f"RSTD{i}", [128, 1], F32) for i in range(4)]
        RT = []
        for i in range(2):
            RT.append(dict(
                EX=sb(f"EX{i}", [128, NE], F32), SC=sb(f"SC{i}", [128, NE], F32),
                SEL=sb(f"SEL{i}", [128, NE], F32), SELP=sb(f"SELP{i}", [128, 4, 8], F32),
                M8=sb(f"M8{i}", [128, 4, 8], F32), GS=sb(f"GS{i}", [128, 4], F32),
                GMX=sb(f"GMX{i}", [128, 1], F32), GM=sb(f"GM{i}", [128, 4], F32),
                MSK=sb(f"MSK{i}", [128, NE], F32), GT=sb(f"GT{i}", [128, NE], F32),
                DEN=sb(f"DEN{i}", [128, 1], F32), RD=sb(f"RD{i}", [128, 1], F32)))
        PB = [es.enter_context(nc.psum_tensor(f"PB{i}", [128, 512], F32)) for i in range(8)]

        engsem = {e: es.enter_context(nc.semaphore(f"s_{e}")) for e in Prog.ENG}
        dma_names = ([f"cast{i}" for i in range(NCAST)] + [f"ring{i}" for i in range(NSLOT)]
                     + [f"xl{i}" for i in range(NCH)] + ["yo0", "yo1", "cst", "lnp", "mem"])
        dmasem = {n: es.enter_context(nc.semaphore(f"d_{n}")) for n in dma_names}

        def bc(t, off, n):
            return bass.AP(t, off, [[0, 128], [1, n]])

        P.add("pool", lambda eng: eng.iota(IOT[:, :], [[1, 128]], base=0, channel_multiplier=-1),
              writes=["IOT"])
        P.add("dve", lambda eng: eng.tensor_scalar(IDF[:, :], IOT[:, :], 0.0, None, op0=ALU.is_equal),
              reads=["IOT"], writes=["IDF"])
        P.add("dve", lambda eng: eng.memset(ONESB[:, :], 1.0), writes=["ONESB"])
        ONESF = IOT[:, :].bitcast(F32)
        P.add("dve", lambda eng: eng.memset(ONESF, 1.0), reads=["IDF"], writes=["IOT"])
        P.add("dve", lambda eng: eng.memset(NEGH[:, :], -0.5), writes=["NEGH"])
        for i in range(2):
            P.add("dve", lambda eng, i=i: eng.memset(RT[i]["SELP"][:, :, :], -1e30), writes=[("SELP", i)])
        P.add("act", lambda eng: eng.dma_start(out=RW[:, :, :],
                                               in_=router_w_t.ap().rearrange("(k p) n -> p k n", p=128)),
              writes=["RW"], dma="cst")
        P.add("act", lambda eng: eng.dma_start(out=RB[:, :], in_=bc(router_b_t, 0, NE)),
              writes=["RB"], dma="cst")
        for l in layers:
            for (dstt, srct, nm) in ((G1T[l], ln1_g_t, "G1T"), (B1T[l], ln1_b_t, "B1T")):
                def col_dma(eng, dstt=dstt, srct=srct, l=l):
                    with nc.allow_non_contiguous_dma(reason="tiny per-partition LN column load"):
                        return eng.dma_start(out=dstt[:, :], in_=srct.ap()[l].rearrange("(k p) -> p k", p=128))
                P.add("act", col_dma, writes=[(nm, l)], dma="cst")
        if 0 in layers:
            P.add("act", lambda eng: eng.dma_start(out=SGUG[:, :], in_=bc(sgu_ln_g_t, 0, 512)),
                  writes=["SGUG"], dma="cst")
            P.add("act", lambda eng: eng.dma_start(out=SGUB[:, :], in_=bc(sgu_ln_b_t, 0, 512)),
                  writes=["SGUB"], dma="cst")
            P.add("act", lambda eng: eng.dma_start(out=BSB[:, :], in_=bc(sgu_b_t, 0, 512)),
                  writes=["BSB"], dma="cst")
            P.add("act", lambda eng: eng.dma_start(
                out=WSF[:, :, :], in_=sgu_w_t.ap()[0].rearrange("h i j -> i h j")),
                writes=["WSF", "Yc"], dma="cst")
            for h in range(4):
                P.add("pe", lambda eng, h=h: eng.transpose(PB[0][:, h * 128:(h + 1) * 128],
                                                           WSF[:, h, :], IDF[:, :]),
                      reads=["WSF", "Yc", "IDF"], writes=[("PB", 0)])
            P.add("dve", lambda eng: eng.tensor_copy(
                WST[:, :, :], PB[0][:, :].rearrange("p (h i) -> p h i", h=4)),
                reads=[("PB", 0)], writes=["WST"])
            P.add("dve", lambda eng: eng.memset(WST[64:128, :, 0:64], 0.0), writes=["WST"])
        if 1 in layers:
            for jj in range(3):
                def cw_dma(eng, jj=jj):
                    with nc.allow_non_contiguous_dma(reason="tiny conv weight transpose"):
                        return eng.dma_start(out=CWT[:, jj, :],
                                             in_=conv_w_t.ap()[0, jj].rearrange("(fc p) -> p fc", p=128))
                P.add("act", cw_dma, writes=["CWT"], dma="cst")

        stream = []
        for sq in range(n_seq):
            for tq in range(tiles_per_seq):
                if tq == 0:
                    for l in layers:
                        stream += [("kv", l, 0), ("kv", l, 1)]
                for l in layers:
                    nin = 3 if l == 0 else 4
                    stream += [("in", l, j) for j in range(nin)]
                    stream += [("out", l, j) for j in range(2)]
                    for e in range(NE):
                        stream += [("g", l, e), ("u", l, e), ("d", l, e)]
        ring = {"next": 0, "released": [False] * len(stream), "cur": 0}

        cast_state = {"done": set(), "ptr": 0, "n": 0}
        CAST_AHEAD = 12

        def cast_upto(i_max):
            while cast_state["ptr"] <= min(i_max, len(stream) - 1):
                key = stream[cast_state["ptr"]]
                cast_state["ptr"] += 1
                u = unit_ids[key]
                if u in cast_state["done"]:
                    continue
                cast_state["done"].add(u)
                src = unit_src[key]
                if key[0] == "d":
                    dst = wbf[u].rearrange("p (mc n) -> p mc n", mc=4)
                else:
                    dst = wbf[u].rearrange("p (kc n) -> p kc n", kc=8)
                P.add("pool", lambda eng, dst=dst, src=src: eng.dma_start(out=dst, in_=src),
                      writes=[("wbf", u)], dma=f"cast{cast_state['n'] % NCAST}")
                cast_state["n"] += 1

        def ring_pump():
            while ring["next"] < len(stream):
                i = ring["next"]
                if i >= NSLOT and not ring["released"][i - NSLOT]:
                    break
                cast_upto(i + CAST_AHEAD)
                s = i % NSLOT
                u = unit_ids[stream[i]]
                P.add("sp", lambda eng, s=s, u=u: eng.dma_start(out=RING[:, s, :], in_=wbf[u]),
                      reads=[("wbf", u)], writes=[("ring", s)], dma=f"ring{s}")
                ring["next"] += 1

        def ring_take(key):
            i = ring["cur"]
            assert stream[i] == key, (stream[i], key)
            ring["cur"] += 1
            if ring["next"] <= i:
                ring_pump()
            assert ring["next"] > i, "ring deadlock"
            return i % NSLOT, i

        def ring_release(i):
            ring["released"][i] = True
            ring_pump()

        def ring_kn(s):
            return RING[:, s, :].rearrange("p (kc n) -> p kc n", kc=8)

        def ring_d(s):
            return RING[:, s, :].rearrange("p (mc n) -> p mc n", mc=4)

        ring_pump()

        cnt = {"ev": 0, "yb": 0, "vg": 0, "u": 0, "pb": 0}

        def next_bank():
            cnt["pb"] += 1
            return cnt["pb"] % 3

        def seq(g):
            for _ in g:
                pass

        def par(*gens):
            act_ = list(gens)
            while act_:
                for g in list(act_):
                    try:
                        next(g)
                    except StopIteration:
                        act_.remove(g)

        def run_tasks(tasks):
            done, active, pending = set(), {}, dict(tasks)

            def start_ready():
                for n in list(pending):
                    g, deps = pending[n]
                    if deps <= done:
                        active[n] = g
                        del pending[n]
            start_ready()
            while active:
                for n in list(active):
                    try:
                        next(active[n])
                    except StopIteration:
                        del active[n]
                        done.add(n)
                        start_ready()
            assert not pending

        def g_tasks(tasks):
            done, active, pending = set(), {}, dict(tasks)

            def start_ready():
                for n in list(pending):
                    g, deps = pending[n]
                    if deps <= done:
                        active[n] = g
                        del pending[n]
            start_ready()
            while active:
                for n in list(active):
                    try:
                        next(active[n])
                    except StopIteration:
                        del active[n]
                        done.add(n)
                        start_ready()
                yield
            assert not pending

        def g_par(*gens):
            act_ = list(gens)
            while act_:
                for g in list(act_):
                    try:
                        next(g)
                    except StopIteration:
                        act_.remove(g)
                yield

        def chain(*gens):
            for g in gens:
                yield from g

        def evac(out, in_, reads, writes):
            cnt["ev"] += 1
            if cnt["ev"] % 2:
                P.add("act", lambda eng: eng.copy(out, in_), reads=reads, writes=writes)
            else:
                P.add("dve", lambda eng: eng.tensor_copy(out, in_), reads=reads, writes=writes)

        def xt_keys(c):
            return [("XT", c, 0), ("XT", c, 1)]

        def g_transposes_to_xt(src, src_key, c, also_f32, banks):
            for kq in range(2):
                bank = banks[kq]
                for j in range(4):
                    k = kq * 4 + j
                    P.add("pe", lambda eng, bank=bank, j=j, k=k: eng.transpose(
                        PB[bank][:, j * 128:(j + 1) * 128], src[:, k * 128:(k + 1) * 128], IDF[:, :]),
                        reads=[src_key, "IDF"], writes=[("PB", bank)])
                yield
                pv = PB[bank][:, :].rearrange("p (j t) -> p j t", j=4)
                P.add("act", lambda eng, pv=pv, kq=kq: eng.copy(
                    XT[:, kq * 4:(kq + 1) * 4, c * 128:(c + 1) * 128], pv),
                    reads=[("PB", bank)], writes=[("XT", c, kq)])
                assert not also_f32
                yield

        def rstd_from_mv(s):
            P.add("pool", lambda eng: eng.tensor_scalar(VEs[s][:, :], MVs[s][:, 1:2], EPS, None, op0=ALU.add),
                  reads=[("MV", s)], writes=[("VE", s)])
            P.add("pool", lambda eng: eng.tensor_tensor(RSTDs[s][:, :], VEs[s][:, :], NEGH[:, :], op=ALU.pow),
                  reads=[("VE", s), "NEGH"], writes=[("RSTD", s)])

        def g_layer_norm_rows(src, dst, src_key, dst_key, gt, bt, gkey, bkey, s):
            for hf in range(2):
                P.add("dve", lambda eng, hf=hf: eng.bn_stats(STs[s][:, hf, :], src[:, hf * 512:(hf + 1) * 512]),
                      reads=[src_key], writes=[("ST", s)])
            P.add("dve", lambda eng: eng.bn_aggr(MVs[s][:, :], STs[s][:, :, :].rearrange("p a b -> p (a b)")),
                  reads=[("ST", s)], writes=[("MV", s)])
            yield
            rstd_from_mv(s)
            yield
            P.add("dve", lambda eng: eng.scalar_tensor_tensor(
                dst, src, MVs[s][:, 0:1], gt[:, :], op0=ALU.subtract, op1=ALU.mult),
                reads=[src_key, ("MV", s), gkey], writes=[dst_key])
            yield
            P.add("dve", lambda eng: eng.scalar_tensor_tensor(
                dst, dst, RSTDs[s][:, 0:1], bt[:, :], op0=ALU.mult, op1=ALU.add),
                reads=[("RSTD", s), bkey], writes=[dst_key])
            yield

        def g_loadx(row0, c, lane):
            P.add("act", lambda eng: eng.dma_start(
                out=R[:, c, :], in_=x_d[row0 + c * 128: row0 + (c + 1) * 128, :]),
                writes=[("R", c)], dma=f"xl{c}")
            yield
            yield from g_transposes_to_xt(R[:, c, :], ("R", c), c, False, [6 + lane, 6 + lane])

        def compute_kv(sq):
            for mc in range(2):
                yb = Yb[mc]
                P.add("act", lambda eng, mc=mc, yb=yb: eng.dma_start(
                    out=yb[:, :], in_=mem_d[sq * N_MEM + mc * 128: sq * N_MEM + (mc + 1) * 128, :]),
                    writes=[("Y", mc)], dma="mem")
                for kq in range(2):
                    bank = 6 + kq
                    for j in range(4):
                        k = kq * 4 + j
                        P.add("pe", lambda eng, bank=bank, j=j, k=k, yb=yb: eng.transpose(
                            PB[bank][:, j * 128:(j + 1) * 128], yb[:, k * 128:(k + 1) * 128], IDF[:, :]),
                            reads=[("Y", mc), "IDF"], writes=[("PB", bank)])
                    pv = PB[bank][:, :].rearrange("p (j t) -> p j t", j=4)
                    evac(MT[:, kq * 4:(kq + 1) * 4, mc * 128:(mc + 1) * 128], pv,
                         [("PB", bank)], [("MT", mc, kq)] + [("ET", a_, b_) for a_ in range(2) for b_ in range(2)])
            mt_keys = [("MT", mc, kq) for mc in range(2) for kq in range(2)]
            for l in layers:
                sk, ik = ring_take(("kv", l, 0))
                sv, iv = ring_take(("kv", l, 1))
                wk, wv = ring_kn(sk), ring_kn(sv)
                for h in range(4):
                    bank = h % 2
                    for k in range(8):
                        P.add("pe", lambda eng, bank=bank, h=h, k=k, wk=wk: eng.matmul(
                            PB[bank][:, 0:N_MEM], lhsT=wk[:, k, h * 128:(h + 1) * 128], rhs=MT[:, k, :],
                            start=(k == 0), stop=(k == 7)),
                            reads=[("ring", sk)] + mt_keys, writes=[("PB", bank)])
                    evac(KT[l][:, h, :], PB[bank][:, 0:N_MEM], [("PB", bank)], [("KT", l)])
                for mc in range(2):
                    bank = mc % 2
                    for k in range(8):
                        P.add("pe", lambda eng, bank=bank, mc=mc, k=k, wv=wv: eng.matmul(
                            PB[bank][:, :], lhsT=MT[:, k, mc * 128:(mc + 1) * 128], rhs=wv[:, k, :],
                            start=(k == 0), stop=(k == 7)),
                            reads=[("ring", sv)] + mt_keys, writes=[("PB", bank)])
                    evac(Vv[l][:, mc, :], PB[bank][:, :], [("PB", bank)], [("V", l)])
                ring_release(ik)
                ring_release(iv)

        def load_ln1_params(l):
            srcs = [ln1_g_t, ln1_b_t]
            for i in range(2):
                P.add("act", lambda eng, i=i: eng.dma_start(out=LNP[i][:, :], in_=bc(srcs[i], l * D, D)),
                      writes=[("LNP", i)], dma="lnp")
                P.add("act", lambda eng, i=i: eng.mul(LNP[i][:, :], LNP[i][:, :], ALPHA), writes=[("LNP", i)])

        def load_ln2_params(l):
            srcs = [ln2_g_t, ln2_b_t]
            for i in range(2):
                P.add("act", lambda eng, i=i: eng.dma_start(out=LNP[2 + i][:, :], in_=bc(srcs[i], l * D, D)),
                      writes=[("LNP", 2 + i)], dma="lnp")

        def g_att_blk(l, cp):
            for h in range(4):
                banks = (4, 5)
                hs = 0
                for mc in range(2):
                    P.add("pe", lambda eng, h=h, mc=mc, banks=banks: eng.matmul(
                        PB[banks[mc]][:, :], lhsT=KT[l][:, h, mc * 128:(mc + 1) * 128], rhs=QT[:, h, :],
                        start=True, stop=True),
                        reads=[("QT", h), ("KT", l)], writes=[("PB", banks[mc])])
                yield
                for mc in range(2):
                    P.add("act", lambda eng, mc=mc, banks=banks, hs=hs: eng.activation(
                        ETall[:, hs, mc, :], PB[banks[mc]][:, :], AF.Exp, scale=SCALE),
                        reads=[("PB", banks[mc])], writes=[("ET", hs, mc)])
                yield
                for mc in range(2):
                    P.add("pe", lambda eng, h=h, mc=mc, banks=banks, hs=hs: eng.matmul(
                        PB[banks[0]][:, :], lhsT=Vv[l][:, mc, h * 128:(h + 1) * 128], rhs=ETall[:, hs, mc, :],
                        start=(mc == 0), stop=(mc == 1)),
                        reads=[("V", l), ("ET", hs, mc)], writes=[("PB", banks[0])])
                for c4 in range(4):
                    for mc in range(2):
                        P.add("pe", lambda eng, c4=c4, mc=mc, banks=banks, hs=hs: eng.matmul(
                            PB[banks[1]][:, c4:c4 + 1], lhsT=ETall[:, hs, mc, c4 * 128:(c4 + 1) * 128],
                            rhs=ONESB[:, 0:1], start=(mc == 0), stop=(mc == 1)),
                            reads=["ONESB", ("ET", hs, mc)], writes=[("PB", banks[1])])
                yield
                P.add("act", lambda eng, banks=banks: eng.copy(PVS, PB[banks[0]][:, :]),
                      reads=[("PB", banks[0])], writes=PVS_KEYS)
                P.add("dve", lambda eng, banks=banks: eng.reciprocal(RT4[:, :], PB[banks[1]][:, 0:4]),
                      reads=[("PB", banks[1])], writes=["RT4"])
                for c4 in range(4):
                    P.add("dve", lambda eng, c4=c4: eng.tensor_scalar(
                        DG[:, c4, :], IDF[:, :], RT4[:, c4:c4 + 1], None, op0=ALU.mult),
                        reads=["IDF", "RT4"], writes=["RDEN"])
                yield
                for c4 in range(4):
                    P.add("pe", lambda eng, c4=c4, banks=banks: eng.matmul(
                        PB[banks[1]][:, c4 * 128:(c4 + 1) * 128], lhsT=ONESF, rhs=DG[:, c4, :],
                        start=True, stop=True),
                        reads=["IOT", "RDEN"], writes=[("PB", banks[1])])
                yield
                P.add("dve", lambda eng, h=h, banks=banks: eng.tensor_tensor(
                    CATTs[cp][:, 4 + h, :], PVS, PB[banks[1]][:, :], op=ALU.mult),
                    reads=PVS_KEYS + [("PB", banks[1])], writes=[("CATT", cp, 4 + h, j) for j in range(4)])
                yield

        def q_proj(blk, wq, sq_slot):
            bs = slice(blk * 512, (blk + 1) * 512)
            xk = [k for c in range(blk * 4, blk * 4 + 4) for k in xt_keys(c)]
            for h in range(4):
                bank = next_bank()
                for k in range(8):
                    P.add("pe", lambda eng, bank=bank, h=h, k=k: eng.matmul(
                        PB[bank][:, :], lhsT=wq[:, k, h * 128:(h + 1) * 128], rhs=XT[:, k, bs],
                        start=(k == 0), stop=(k == 7)),
                        reads=[("ring", sq_slot)] + xk, writes=[("PB", bank)])
                evac(QT[:, h, :], PB[bank][:, :], [("PB", bank)], [("QT", h)])
                yield

        def g_out_a(c, j, wo, so, lane, cp):
            Y, ykey, bank = Yb[lane], ("Y", lane), 6 + lane
            cs = slice(j * 128, (j + 1) * 128)
            catt_keys = [("CATT", cp, fc, j) for fc in range(8)]
            for hf in range(2):
                for fc in range(8):
                    P.add("pe", lambda eng, fc=fc, hf=hf: eng.matmul(
                        PB[bank][:, :], lhsT=CATTs[cp][:, fc, cs], rhs=wo[hf][:, fc, :],
                        start=(fc == 0), stop=(fc == 7)),
                        reads=catt_keys + [("ring", so[hf])], writes=[("PB", bank)])
                yield
                P.add("dve", lambda eng, hf=hf: eng.scalar_tensor_tensor(
                    Y[:, hf * 512:(hf + 1) * 512], R[:, c, hf * 512:(hf + 1) * 512], ALPHA, PB[bank][:, :],
                    op0=ALU.mult, op1=ALU.add),
                    reads=[("R", c), ("PB", bank)], writes=[ykey])
                yield

        def g_out_b(l, c, lane):
            Y, ykey, bank = Yb[lane], ("Y", lane), 6 + lane
            X1T, NMR, T = X1Ts[lane], NMRs[lane], RT[lane]
            s0 = lane
            tk = lambda n: (n, lane)
            for hf in range(2):
                P.add("dve", lambda eng, hf=hf: eng.bn_stats(STs[s0][:, hf, :], Y[:, hf * 512:(hf + 1) * 512]),
                      reads=[ykey], writes=[("ST", s0)])
            P.add("dve", lambda eng: eng.bn_aggr(MVs[s0][:, :], STs[s0][:, :, :].rearrange("p a b -> p (a b)")),
                  reads=[("ST", s0)], writes=[("MV", s0)])
            yield
            rstd_from_mv(s0)
            yield
            P.add("dve", lambda eng: eng.tensor_scalar(NMR[:, :], MVs[s0][:, 0:1], RSTDs[s0][:, 0:1], -1.0,
                                                       op0=ALU.mult, op1=ALU.mult),
                  reads=[("MV", s0), ("RSTD", s0)], writes=[tk("NMR")])
            yield
            P.add("act", lambda eng: eng.activation(Y[:, :], Y[:, :], AF.Identity,
                                                    bias=NMR[:, 0:1], scale=RSTDs[s0][:, 0:1]),
                  reads=[tk("NMR"), ("RSTD", s0)], writes=[ykey])
            yield
            for kq in range(2):
                for j in range(4):
                    k = kq * 4 + j
                    P.add("pe", lambda eng, j=j, k=k: eng.transpose(
                        PB[bank][:, j * 128:(j + 1) * 128], Y[:, k * 128:(k + 1) * 128], IDF[:, :]),
                        reads=[ykey, "IDF"], writes=[("PB", bank)])
                yield
                for j in range(4):
                    k = kq * 4 + j
                    P.add("act", lambda eng, j=j, k=k: eng.activation(
                        X1T[:, k, :], PB[bank][:, j * 128:(j + 1) * 128], AF.Identity,
                        bias=B1T[l][:, k:k + 1], scale=G1T[l][:, k:k + 1]),
                        reads=[("PB", bank), ("G1T", l), ("B1T", l)], writes=[("X1T", lane, kq)])
                yield
                P.add("act", lambda eng, kq=kq: eng.copy(
                    XT[:, kq * 4:(kq + 1) * 4, c * 128:(c + 1) * 128], X1T[:, kq * 4:(kq + 1) * 4, :]),
                    reads=[("X1T", lane, kq)], writes=[("XT", c, kq)])
                yield
            for k in range(8):
                P.add("pe", lambda eng, k=k: eng.matmul(
                    PB[bank][:, 0:NE], lhsT=X1T[:, k, :], rhs=RW[:, k, :], start=(k == 0), stop=(k == 7)),
                    reads=[("X1T", lane, 0), ("X1T", lane, 1), "RW"], writes=[("PB", bank)])
            yield
            EX, SC, SEL, SELP, M8, GS = T["EX"], T["SC"], T["SEL"], T["SELP"], T["M8"], T["GS"]
            GMX, GM, MSK, GT, DEN, RD = T["GMX"], T["GM"], T["MSK"], T["GT"], T["DEN"], T["RD"]
            P.add("act", lambda eng: eng.activation(EX[:, :], PB[bank][:, 0:NE], AF.Exp, scale=-1.0),
                  reads=[("PB", bank)], writes=[tk("EX")])
            yield
            P.add("dve", lambda eng: eng.tensor_scalar(SC[:, :], EX[:, :], 1.0, None, op0=ALU.add),
                  reads=[tk("EX")], writes=[tk("SC")])
            P.add("dve", lambda eng: eng.reciprocal(SC[:, :], SC[:, :]), writes=[tk("SC")])
            P.add("dve", lambda eng: eng.tensor_tensor(SEL[:, :], SC[:, :], RB[:, :], op=ALU.add),
                  reads=[tk("SC"), "RB"], writes=[tk("SEL")])
            yield
            sel3 = SEL[:, :].rearrange("p (g k) -> p g k", g=4)
            P.add("dve", lambda eng: eng.tensor_copy(SELP[:, :, 0:4], sel3), reads=[tk("SEL")], writes=[tk("SELP")])
            for g in range(4):
                P.add("dve", lambda eng, g=g: eng.max(M8[:, g, :], SELP[:, g, :]),
                      reads=[tk("SELP")], writes=[tk("M8")])
            yield
            P.add("dve", lambda eng: eng.tensor_tensor(GS[:, :], M8[:, :, 0], M8[:, :, 1], op=ALU.add),
                  reads=[tk("M8")], writes=[tk("GS")])
            P.add("dve", lambda eng: eng.tensor_reduce(GMX[:, :], GS[:, :], axis=AX.X, op=ALU.max),
                  reads=[tk("GS")], writes=[tk("GMX")])
            P.add("dve", lambda eng: eng.tensor_scalar(GM[:, :], GS[:, :], GMX[:, 0:1], None, op0=ALU.is_ge),
                  reads=[tk("GS"), tk("GMX")], writes=[tk("GM")])
            yield
            msk3 = MSK[:, :].rearrange("p (g k) -> p g k", g=4)
            P.add("dve", lambda eng: eng.tensor_tensor(
                msk3, sel3, M8[:, :, 1:2].to_broadcast([128, 4, 4]), op=ALU.is_ge),
                reads=[tk("SEL"), tk("M8")], writes=[tk("MSK")])
            P.add("dve", lambda eng: eng.tensor_tensor(
                msk3, msk3, GM[:, :].unsqueeze(2).to_broadcast([128, 4, 4]), op=ALU.mult),
                reads=[tk("GM")], writes=[tk("MSK")])
            P.add("dve", lambda eng: eng.tensor_tensor(GT[:, :], MSK[:, :], SC[:, :], op=ALU.mult),
                  reads=[tk("MSK"), tk("SC")], writes=[tk("GT")])
            yield
            P.add("dve", lambda eng: eng.tensor_reduce(DEN[:, :], GT[:, :], axis=AX.X, op=ALU.add),
                  reads=[tk("GT")], writes=[tk("DEN")])
            P.add("dve", lambda eng: eng.reciprocal(RD[:, :], DEN[:, :]), reads=[tk("DEN")], writes=[tk("RD")])
            P.add("dve", lambda eng: eng.tensor_scalar(CW[:, c, :], GT[:, :], RD[:, 0:1], None, op0=ALU.mult),
                  reads=[tk("GT"), tk("RD")], writes=[("CW", c)])
            yield
            P.add("dve", lambda eng: eng.tensor_tensor(R[:, c, :], Y[:, :], LNP[0][:, :], op=ALU.mult),
                  reads=[ykey, ("LNP", 0)], writes=[("R", c)])
            yield
            P.add("dve", lambda eng: eng.tensor_tensor(R[:, c, :], R[:, c, :], LNP[1][:, :], op=ALU.add),
                  reads=[("LNP", 1)], writes=[("R", c)])
            yield

        def g_phase1_a(l, blk, W, cp, do_q=True):
            wu, wv, wq, su, sv_, sq_ = W
            bs = slice(blk * 512, (blk + 1) * 512)
            xk = [k for c in range(blk * 4, blk * 4 + 4) for k in xt_keys(c)]
            for j in range(4):
                c = blk * 4 + j
                bank = next_bank()
                vg_i = cnt["vg"] % 2
                cnt["vg"] += 1
                vg = VG[vg_i]
                s = 2 + vg_i
                for k in range(8):
                    P.add("pe", lambda eng, bank=bank, k=k, c=c: eng.matmul(
                        PB[bank][:, :], lhsT=XT[:, k, c * 128:(c + 1) * 128], rhs=wv[:, k, :],
                        start=(k == 0), stop=(k == 7)),
                        reads=xt_keys(c) + [("ring", sv_)], writes=[("PB", bank)])
                P.add("act", lambda eng, bank=bank, vg=vg: eng.activation(vg[:, :], PB[bank][:, :], AF.Gelu),
                      reads=[("PB", bank)], writes=[("VG", vg_i)])
                yield
                P.add("dve", lambda eng, vg=vg, s=s: eng.bn_stats(STs[s][:, 0, :], vg[:, :]),
                      reads=[("VG", vg_i)], writes=[("ST", s)])
                P.add("dve", lambda eng, s=s: eng.bn_aggr(MVs[s][:, :], STs[s][:, 0, :]),
                      reads=[("ST", s)], writes=[("MV", s)])
                rstd_from_mv(s)
                yield
                P.add("dve", lambda eng, vg=vg, s=s: eng.scalar_tensor_tensor(
                    vg[:, :], vg[:, :], MVs[s][:, 0:1], SGUG[:, :], op0=ALU.subtract, op1=ALU.mult),
                    reads=[("MV", s), "SGUG"], writes=[("VG", vg_i)])
                P.add("dve", lambda eng, vg=vg, j=j, s=s: eng.scalar_tensor_tensor(
                    VN[:, j, :], vg[:, :], RSTDs[s][:, 0:1], SGUB[:, :], op0=ALU.mult, op1=ALU.add),
                    reads=[("VG", vg_i), ("RSTD", s), "SGUB"], writes=[("VN", j)])
                yield
            if do_q:
                yield from q_proj(blk, wq, sq_)
            for h in range(4):
                bank = next_bank()
                u_i = cnt["u"] % 2
                cnt["u"] += 1
                U = Ub[u_i]
                for k in range(8):
                    P.add("pe", lambda eng, bank=bank, h=h, k=k: eng.matmul(
                        PB[bank][:, :], lhsT=wu[:, k, h * 128:(h + 1) * 128], rhs=XT[:, k, bs],
                        start=(k == 0), stop=(k == 7)),
                        reads=[("ring", su)] + xk, writes=[("PB", bank)])
                P.add("act", lambda eng, bank=bank, U=U: eng.activation(U[:, :], PB[bank][:, :], AF.Gelu),
                      reads=[("PB", bank)], writes=[("U", u_i)])
                yield
                for j in range(4):
                    P.add("pe", lambda eng, h=h, j=j: eng.matmul(
                        PB[3][:, j * 128:(j + 1) * 128], lhsT=VN[:, j, h * 128:(h + 1) * 128],
                        rhs=WST[:, h, :], start=True, stop=True),
                        reads=[("VN", j), "WST"], writes=[("PB", 3)])
                yield
                P.add("dve", lambda eng, h=h: eng.tensor_tensor(
                    Tt[:, :].rearrange("p (j i) -> p j i", j=4),
                    PB[3][:, :].rearrange("p (j i) -> p j i", j=4),
                    BSB[:, h * 128:(h + 1) * 128].unsqueeze(1).to_broadcast([128, 4, 128]), op=ALU.add),
                    reads=[("PB", 3), "BSB"], writes=["Tt"])
                P.add("dve", lambda eng, h=h, U=U: eng.tensor_tensor(
                    CATTs[cp][:, h, :], Tt[:, :], U[:, :], op=ALU.mult),
                    reads=["Tt", ("U", u_i)], writes=[("CATT", cp, h, j) for j in range(4)])
                yield

        def g_phase1_b(l, blk, W, cp, do_q=True):
            wb, wc, wx, wq, sb_, sc_, sx_, sq_ = W
            bs = slice(blk * 512, (blk + 1) * 512)
            xk = [k for c in range(blk * 4, blk * 4 + 4) for k in xt_keys(c)]
            if do_q:
                yield from q_proj(blk, wq, sq_)
            for fc in range(4):
                fs = slice(fc * 128, (fc + 1) * 128)
                bb, bcg, bx = (0, 1, 2) if fc % 2 == 0 else (3, 1, 2)
                for (w, sl, bank) in ((wb, sb_, bb), (wc, sc_, bcg), (wx, sx_, bx)):
                    for k in range(8):
                        P.add("pe", lambda eng, w=w, bank=bank, k=k, fs=fs: eng.matmul(
                            PB[bank][:, :], lhsT=w[:, k, fs], rhs=XT[:, k, bs],
                            start=(k == 0), stop=(k == 7)),
                            reads=[("ring", sl)] + xk, writes=[("PB", bank)])
                    yield
                P.add("act", lambda eng, bcg=bcg: eng.copy(CGs[:, :], PB[bcg][:, :]), reads=[("PB", bcg)], writes=["Tt"])
                P.add("act", lambda eng, fc=fc: eng.copy(HTw[:, 0:2], HS[:, fc, :]),
                      reads=[("HS", fc)], writes=["HTw"])
                yield
                P.add("dve", lambda eng, bx=bx: eng.tensor_tensor(
                    HTw[:, 2:514], CGs[:, :], PB[bx][:, :], op=ALU.mult),
                    reads=["Tt", ("PB", bx)], writes=["HTw"])
                P.add("dve", lambda eng, fc=fc: eng.tensor_scalar(
                    Yc[:, :], HTw[:, 2:514], CWT[:, 2, fc:fc + 1], None, op0=ALU.mult),
                    reads=["HTw", "CWT"], writes=["Yc"])
                yield
                P.add("dve", lambda eng, fc=fc: eng.scalar_tensor_tensor(
                    Yc[:, :], HTw[:, 1:513], CWT[:, 1, fc:fc + 1], Yc[:, :], op0=ALU.mult, op1=ALU.add),
                    reads=["HTw", "CWT"], writes=["Yc"])
                P.add("dve", lambda eng, fc=fc: eng.scalar_tensor_tensor(
                    Yc[:, :], HTw[:, 0:512], CWT[:, 0, fc:fc + 1], Yc[:, :], op0=ALU.mult, op1=ALU.add),
                    reads=["HTw", "CWT"], writes=["Yc"])
                yield
                P.add("dve", lambda eng, fc=fc, bb=bb: eng.tensor_tensor(
                    CATTs[cp][:, fc, :], Yc[:, :], PB[bb][:, :], op=ALU.mult),
                    reads=["Yc", ("PB", bb)], writes=[("CATT", cp, fc, j) for j in range(4)])
                P.add("act", lambda eng, fc=fc: eng.copy(HS[:, fc, :], HTw[:, 512:514]),
                      reads=["HTw"], writes=[("HS", fc)])
                yield

        def mixer(l, first_tile_of_seq, pre_gens):
            nin = 3 if l == 0 else 4
            slots = [ring_take(("in", l, j)) for j in range(nin)]
            oslots = [ring_take(("out", l, j)) for j in range(2)]
            W = tuple(ring_kn(s[0]) for s in slots) + tuple(s[0] for s in slots)
            wo = [ring_kn(oslots[0][0]), ring_kn(oslots[1][0])]
            so = [oslots[0][0], oslots[1][0]]
            ph1 = g_phase1_a if l == 0 else g_phase1_b
            if l == 1 and first_tile_of_seq:
                P.add("dve", lambda eng: eng.memset(HS[:, :, :], 0.0), writes=[("HS", fc) for fc in range(4)])
            def stream_a(blk):
                wq_, sq_ = (W[2], W[5]) if l == 0 else (W[3], W[7])
                return chain(q_proj(blk, wq_, sq_),
                             g_par(g_att_blk(l, blk % 2), ph1(l, blk, W, blk % 2, do_q=False)))

            def stream_b(blk):
                tasks = {}
                for j in range(4):
                    c = blk * 4 + j
                    deps = {f"O{j - 2}"} if j >= 2 else set()
                    tasks[f"O{j}"] = (chain(g_out_a(c, j, wo, so, j % 2, blk % 2), g_out_b(l, c, j % 2)), deps)
                return g_tasks(tasks)

            par(*pre_gens, stream_a(0))
            for blk in range(1, NBLK):
                par(stream_a(blk), stream_b(blk - 1))
            for s in slots:
                ring_release(s[1])

            def release_out():
                for s in oslots:
                    ring_release(s[1])
            return stream_b(NBLK - 1), release_out

        def moe(l, tail, release_out):
            assert NBLK == 2
            items = [(0, 0), (1, 0), (0, 1), (1, 1)] + [(e, blk) for e in range(2, NE) for blk in range(NBLK)]
            last_item = {}
            for i, (e, _) in enumerate(items):
                last_item[e] = i
            order = [(k, e) for e in range(NE) for k in ("g", "u", "d")]
            taken = {}
            st = {"ptr": 0}

            def need(kind, e):
                tgt = order.index((kind, e))
                while st["ptr"] <= tgt:
                    k2, e2 = order[st["ptr"]]
                    taken[(k2, e2)] = ring_take((k2, l, e2))
                    st["ptr"] += 1
                return taken[(kind, e)]

            def g_p1(idx):
                e, blk = items[idx]
                su, _ = need("u", e)
                sg, _ = taken[("g", e)]
                wg, wu = ring_kn(sg), ring_kn(su)
                bs = slice(blk * 512, (blk + 1) * 512)
                xk = [k for c in range(blk * 4, blk * 4 + 4) for k in xt_keys(c)]
                hT = HTb[idx % 2]
                for m in range(4):
                    ms = slice(m * 128, (m + 1) * 128)
                    bg, bu = m % 2, 2 + m % 2
                    for k in range(8):
                        P.add("pe", lambda eng, bg=bg, k=k, ms=ms: eng.matmul(
                            PB[bg][:, :], lhsT=wg[:, k, ms], rhs=XT[:, k, bs], start=(k == 0), stop=(k == 7)),
                            reads=[("ring", sg)] + xk, writes=[("PB", bg)])
                    yield
                    for k in range(8):
                        P.add("pe", lambda eng, bu=bu, k=k, ms=ms: eng.matmul(
                            PB[bu][:, :], lhsT=wu[:, k, ms], rhs=XT[:, k, bs], start=(k == 0), stop=(k == 7)),
                            reads=[("ring", su)] + xk, writes=[("PB", bu)])
                    sgt = SG[m % 2]
                    P.add("act", lambda eng, bg=bg, sgt=sgt: eng.activation(sgt[:, :], PB[bg][:, :], AF.Silu),
                          reads=[("PB", bg)], writes=[("SG", m % 2)])
                    P.add("dve", lambda eng, bu=bu, sgt=sgt, m=m: eng.tensor_tensor(
                        hT[:, m, :], sgt[:, :], PB[bu][:, :], op=ALU.mult),
                        reads=[("SG", m % 2), ("PB", bu)], writes=[("hT", idx % 2, m)])
                    yield

            def g_p2(idx, banks):
                e, blk = items[idx]
                sd, _ = need("d", e)
                wd = ring_d(sd)
                hT = HTb[idx % 2]
                hk = [("hT", idx % 2, m) for m in range(4)]
                nb = len(banks)
                for s in range(4):
                    c = blk * 4 + s
                    for hf in range(2):
                        bank = banks[(s * 2 + hf) % nb]
                        for m in range(4):
                            P.add("pe", lambda eng, bank=bank, m=m, s=s, hf=hf: eng.matmul(
                                PB[bank][:, :], lhsT=hT[:, m, s * 128:(s + 1) * 128],
                                rhs=wd[:, m, hf * 512:(hf + 1) * 512], start=(m == 0), stop=(m == 3)),
                                reads=hk + [("ring", sd)], writes=[("PB", bank)])
                        P.add("dve", lambda eng, bank=bank, c=c, hf=hf: eng.scalar_tensor_tensor(
                            R[:, c, hf * 512:(hf + 1) * 512], PB[bank][:, :], CW[:, c, e:e + 1],
                            R[:, c, hf * 512:(hf + 1) * 512], op0=ALU.mult, op1=ALU.add),
                            reads=[("PB", bank), ("CW", c)], writes=[("R", c)])
                        yield
                if idx == last_item[e]:
                    for k in ("g", "u", "d"):
                        ring_release(taken[(k, e)][1])

            par(tail, chain(g_p1(0), g_p1(1), g_p2(0, [4, 5])))
            release_out()
            load_ln2_params(l)
            n = len(items)
            for i in range(2, n - 1):
                seq(g_p1(i))
                seq(g_p2(i - 1, [4, 5, 6, 7]))
            seq(g_p2(n - 2, [4, 5, 6, 7]))
            return chain(g_p1(n - 1), g_p2(n - 1, [4, 5]))

        def g_ln2(c, lane, last, row0, next_row0):
            if last:
                Y = Yb[lane]
                yield from g_layer_norm_rows(R[:, c, :], Y[:, :], ("R", c), ("Y", lane), LNP[2], LNP[3],
                                             ("LNP", 2), ("LNP", 3), lane)
                P.add("act", lambda eng: eng.dma_start(
                    out=y_d[row0 + c * 128: row0 + (c + 1) * 128, :], in_=Y[:, :]),
                    reads=[("Y", lane)], dma=f"yo{lane}")
                yield
                if next_row0 is not None:
                    yield from g_loadx(next_row0, c, lane)
            else:
                yield from g_layer_norm_rows(R[:, c, :], R[:, c, :], ("R", c), ("R", c), LNP[2], LNP[3],
                                             ("LNP", 2), ("LNP", 3), lane)
                yield from g_transposes_to_xt(R[:, c, :], ("R", c), c, False, [6 + lane, 6 + lane])

        def ln2_stage(last, row0, next_row0, defer, moe_tail):
            def lanes(cs):
                return [chain(*[g_ln2(c, lane, last, row0, next_row0) for c in cs if c % 2 == lane])
                        for lane in range(2)]
            par(*lanes(range(0, 4)), moe_tail)
            if defer:
                return lanes(range(4, NCH))
            par(*lanes(range(4, NCH)))
            return []

        tiles = [(sq, tq) for sq in range(n_seq) for tq in range(tiles_per_seq)]
        par(*[chain(*[g_loadx(0, c, lane) for c in range(lane, NCH, 2)]) for lane in range(2)])
        pre = []
        for ti, (sq, tq) in enumerate(tiles):
            row0 = sq * seq_len + tq * TT
            next_row0 = None
            if ti + 1 < len(tiles):
                nsq, ntq = tiles[ti + 1]
                next_row0 = nsq * seq_len + ntq * TT
            if tq == 0:
                assert not pre
                compute_kv(sq)
            for li, l in enumerate(layers):
                last = li == len(layers) - 1
                load_ln1_params(l)
                tail, release_out = mixer(l, tq == 0, pre)
                moe_tail = moe(l, tail, release_out)
                if last:
                    defer = (ti + 1 < len(tiles)) and tiles[ti + 1][1] != 0
                else:
                    defer = True
                pre = ln2_stage(last, row0, next_row0, defer, moe_tail)

        P.emit(nc, engsem, dmasem)
    return nc


_WKEYS = ["w_in_a", "sgu_ln_g", "sgu_ln_b", "sgu_w", "sgu_b", "w_in_b", "conv_w", "w_kv", "w_out",
          "ln1_g", "ln1_b", "router_w", "router_b", "w_gate", "w_up", "w_down", "ln2_g", "ln2_b"]


def _in_map(x_c, mem_c, inputs):
    m = {"x": np.ascontiguousarray(x_c.reshape(-1, D), dtype=np.float32),
         "mem": np.ascontiguousarray(mem_c.reshape(-1, D), dtype=np.float32)}
    for k in _WKEYS:
        a = np.ascontiguousarray(np.asarray(inputs[k], dtype=np.float32))
        if k == "sgu_b":
            a = a.reshape(1, 512)
        if k == "router_b":
            a = a.reshape(1, NE)
        m[k] = a
    return m


def run(inputs, n_cores, layers=(0, 1), trace=False, dbg=None):
    x = np.asarray(inputs["x"], dtype=np.float32)
    mem = np.asarray(inputs["mem"], dtype=np.float32)
    B, S, _ = x.shape
    n_seq = B // n_cores
    nc = build(n_seq, S, list(layers), dbg=dbg)
    in_maps = [_in_map(x[i * n_seq:(i + 1) * n_seq], mem[i * n_seq:(i + 1) * n_seq], inputs)
               for i in range(n_cores)]
    res = run_bass_kernel_spmd(nc, in_maps, core_ids=list(range(n_cores)), trace=trace)
    out = np.concatenate([np.asarray(r["y"]).reshape(n_seq, S, D) for r in res.results], axis=0)
    return out.astype(np.float32), res


def kernel(**inputs):
    out, _ = run(inputs, 8)
    return out
```

```python
import numpy as np
import concourse.bass as bass
import concourse.mybir as mybir
from concourse.bass_utils import run_bass_kernel_spmd

F32 = mybir.dt.float32
BF16 = mybir.dt.bfloat16
I32 = mybir.dt.int32
AF = mybir.ActivationFunctionType
ALU = mybir.AluOpType
AX = mybir.AxisListType

D = 1024
MIXW = 512
N_MEM = 256
NE = 16
DEPTH = 2
ALPHA = float((2 * DEPTH) ** 0.25)
EPS = 1e-5
SCALE = float(128 ** -0.5)
TT = 1024
NCH = TT // 128
NBLK = TT // 512
NSLOT = 7
NCAST = 8


class Op:
    __slots__ = ("eng", "fn", "dma", "deps", "needed", "sig")

    def __init__(self, eng, fn, dma):
        self.eng = eng
        self.fn = fn
        self.dma = dma
        self.deps = ()
        self.needed = False
        self.sig = None


class Prog:
    ENG = ("pe", "act", "dve", "pool", "sp")

    def __init__(self):
        self.ops = {e: [] for e in self.ENG}
        self.lastw = {}
        self.readers = {}
        self.dma_last = {}
        self.final = []

    def add(self, eng, fn, reads=(), writes=(), dma=None):
        op = Op(eng, fn, dma)
        deps = {}
        wset = set(writes)
        for r in reads:
            if isinstance(r, tuple) and r[0] == "PB":
                wset.add(r)
        for r in reads:
            w = self.lastw.get(r)
            if w is not None:
                deps[id(w)] = w
        for r in wset:
            w = self.lastw.get(r)
            if w is not None:
                deps[id(w)] = w
            rd = self.readers.get(r)
            if rd:
                for o in rd.values():
                    deps[id(o)] = o
        if dma is not None:
            prev = self.dma_last.get(dma)
            if prev is not None:
                deps[id(prev)] = prev
            self.dma_last[dma] = op
        op.deps = [d for d in deps.values()
                   if not (d.eng == "pe" and eng == "pe" and d.dma is None and dma is None)]
        for r in wset:
            self.lastw[r] = op
            self.readers[r] = {}
        for r in reads:
            if r not in wset:
                self.readers.setdefault(r, {})[eng] = op
        self.ops[eng].append(op)
        return op

    def emit(self, nc, engsem, dmasem):
        for e in self.ENG:
            for op in self.ops[e]:
                for d in op.deps:
                    d.needed = True
        dcnt = {}
        for e in self.ENG:
            cnt = 0
            for op in self.ops[e]:
                if op.dma is not None:
                    dcnt[op.dma] = dcnt.get(op.dma, 0) + 16
                    op.sig = (("dma", op.dma), dcnt[op.dma])
                elif op.needed:
                    cnt += 1
                    op.sig = (("eng", e), cnt)
        finals = [(("dma", k), v) for k, v in dcnt.items()]

        def handle(key):
            return dmasem[key[1]] if key[0] == "dma" else engsem[key[1]]

        def run(e, eng):
            seen = {}
            for op in self.ops[e]:
                for d in op.deps:
                    key, val = d.sig
                    if seen.get(key, 0) >= val:
                        continue
                    eng.wait_ge(handle(key), val)
                    seen[key] = val
                ins = op.fn(eng)
                if op.dma is not None:
                    ins.then_inc(dmasem[op.dma], 16)
                elif op.needed:
                    ins.then_inc(engsem[e], 1)
            if e == "sp":
                for key, val in finals:
                    if seen.get(key, 0) < val:
                        eng.wait_ge(handle(key), val)

        with nc.Block() as block:
            @block.tensor
            def _(eng):
                run("pe", eng)

            @block.scalar
            def _(eng):
                run("act", eng)

            @block.vector
            def _(eng):
                run("dve", eng)

            @block.gpsimd
            def _(eng):
                run("pool", eng)

            @block.sync
            def _(eng):
                run("sp", eng)


def build(n_seq, seq_len, layers, dbg=None):
    assert seq_len % TT == 0
    tiles_per_seq = seq_len // TT
    ntok = n_seq * seq_len
    nc = bass.Bass("TRN2", target_bir_lowering=False)

    def din(name, shape):
        return nc.dram_tensor(name, list(shape), F32, kind="ExternalInput")

    x_t = din("x", [ntok, D])
    mem_t = din("mem", [n_seq * N_MEM, D])
    w_in_a_t = din("w_in_a", [1, D, 3 * MIXW])
    sgu_ln_g_t = din("sgu_ln_g", [1, MIXW])
    sgu_ln_b_t = din("sgu_ln_b", [1, MIXW])
    sgu_w_t = din("sgu_w", [1, 4, 128, 128])
    sgu_b_t = din("sgu_b", [1, 4 * 128])
    w_in_b_t = din("w_in_b", [1, D, 4 * MIXW])
    conv_w_t = din("conv_w", [1, 3, MIXW])
    w_kv_t = din("w_kv", [2, D, 2 * MIXW])
    w_out_t = din("w_out", [2, D, D])
    ln1_g_t = din("ln1_g", [2, D])
    ln1_b_t = din("ln1_b", [2, D])
    router_w_t = din("router_w", [D, NE])
    router_b_t = din("router_b", [1, NE])
    w_gate_t = din("w_gate", [2, NE, D, MIXW])
    w_up_t = din("w_up", [2, NE, D, MIXW])
    w_down_t = din("w_down", [2, NE, MIXW, D])
    ln2_g_t = din("ln2_g", [2, D])
    ln2_b_t = din("ln2_b", [2, D])
    y_t = nc.dram_tensor("y", [ntok, D], F32, kind="ExternalOutput")

    x_d, mem_d, y_d = x_t.ap(), mem_t.ap(), y_t.ap()

    unit_src = {}
    unit_ids = {}

    def reg_unit(key, src_ap):
        unit_ids[key] = len(unit_ids)
        unit_src[key] = src_ap

    def kn(ap2d, j):
        return ap2d[:, j * 512:(j + 1) * 512].rearrange("(kc p) n -> p kc n", p=128)

    for l in (0, 1):
        for j in range(2):
            reg_unit(("kv", l, j), kn(w_kv_t.ap()[l], j))
    for l in layers:
        nin = 3 if l == 0 else 4
        wi = w_in_a_t.ap()[0] if l == 0 else w_in_b_t.ap()[0]
        for j in range(nin):
            reg_unit(("in", l, j), kn(wi, j))
        for j in range(2):
            reg_unit(("out", l, j), kn(w_out_t.ap()[l], j))
        for e in range(NE):
            reg_unit(("g", l, e), kn(w_gate_t.ap()[l, e], 0))
            reg_unit(("u", l, e), kn(w_up_t.ap()[l, e], 0))
            reg_unit(("d", l, e), w_down_t.ap()[l, e].rearrange("(mc p) n -> p mc n", p=128))
    NU = len(unit_ids)
    wbf_t = nc.dram_tensor("wbf", [NU, 128, 4096], BF16, kind="Internal")
    wbf = wbf_t.ap()

    P = Prog()
    import contextlib
    es = contextlib.ExitStack()

    def sb(name, shape, dt):
        return es.enter_context(nc.sbuf_tensor(name, list(shape), dt))

    with es:
        R = sb("R", [128, NCH, D], F32)
        XT = sb("XT", [128, 8, TT], BF16)
        RING = sb("RING", [128, NSLOT, 4096], BF16)
        HTb = [sb(f"hT{i}", [128, 4, 512], BF16) for i in range(2)]
        SG = [sb(f"SG{i}", [128, 512], F32) for i in range(2)]
        CATTs = [sb(f"CATT{i}", [128, 8, 512], BF16) for i in range(2)]
        Ub = [sb(f"U{i}", [128, 512], F32) for i in range(2)]
        Tt = sb("Tt", [128, 512], F32)
        CGs = Tt
        QT = sb("QT", [128, 4, 512], BF16)
        VN = sb("VN", [128, 4, 512], BF16)
        VG = [sb(f"VG{i}", [128, 512], F32) for i in range(2)]
        Yc = VG[1][:, :]
        HTw = VN[:, :, :].rearrange("p a b -> p (a b)").bitcast(F32)[:, 0:516]
        ETall = sb("ETall", [128, 2, 2, 512], BF16)
        MT = ETall[:, :, :, :].rearrange("p a b c -> p (a b c)").rearrange("p (k m) -> p k m", k=8)
        PVS = ETall[:, 1, :, :].rearrange("p b c -> p (b c)").bitcast(F32)
        PVS_KEYS = [("ET", 1, 0), ("ET", 1, 1)]
        RDEN = sb("RDEN", [128, 512], F32)
        DG = RDEN[:, :].rearrange("p (c t) -> p c t", c=4)
        RT4 = sb("RT4", [128, 4], F32)
        ONESB = sb("ONESB", [128, 128], BF16)
        Yb = [sb(f"Y{i}", [128, D], F32) for i in range(2)]
        X1Ts = [sb(f"X1T{i}", [128, 8, 128], F32) for i in range(2)]
        HS = sb("HS", [128, 4, 2], F32)
        KT = [sb(f"KT{l}", [128, 4, N_MEM], BF16) for l in range(2)]
        Vv = [sb(f"V{l}", [128, 2, 512], BF16) for l in range(2)]
        LNP = [sb(f"LNP{i}", [128, D], F32) for i in range(4)]
        G1T = [sb(f"G1T{l}", [128, 8], F32) for l in range(2)]
        B1T = [sb(f"B1T{l}", [128, 8], F32) for l in range(2)]
        NMRs = [sb(f"NMR{i}", [128, 1], F32) for i in range(2)]
        SGUG = sb("SGUG", [128, 512], F32)
        SGUB = sb("SGUB", [128, 512], F32)
        BSB = sb("BSB", [128, 512], F32)
        WST = sb("WST", [128, 4, 128], BF16)
        WSF = Yc[:, :].rearrange("p (h j) -> p h j", h=4)
        CWT = sb("CWT", [128, 3, 4], F32)
        RW = sb("RW", [128, 8, NE], F32)
        RB = sb("RB", [128, NE], F32)
        CW = sb("CW", [128, NCH, NE], F32)
        IDF = sb("IDF", [128, 128], F32)
        IOT = sb("IOT", [128, 128], I32)
        NEGH = sb("NEGH", [128, 1], F32)
        STs = [sb(f"ST{i}", [128, 2, 6], F32) for i in range(4)]
        MVs = [sb(f"MV{i}", [128, 2], F32) for i in range(4)]
        VEs = [sb(f"VE{i}", [128, 1], F32) for i in range(4)]
        RSTDs = [sb(f"RSTD{i}", [128, 1], F32) for i in range(4)]
        RT = []
        for i in range(2):
            RT.append(dict(
                EX=sb(f"EX{i}", [128, NE], F32), SC=sb(f"SC{i}", [128, NE], F32),
                SEL=sb(f"SEL{i}", [128, NE], F32), SELP=sb(f"SELP{i}", [128, 4, 8], F32),
                M8=sb(f"M8{i}", [128, 4, 8], F32), GS=sb(f"GS{i}", [128, 4], F32),
                GMX=sb(f"GMX{i}", [128, 1], F32), GM=sb(f"GM{i}", [128, 4], F32),
                MSK=sb(f"MSK{i}", [128, NE], F32), GT=sb(f"GT{i}", [128, NE], F32),
                DEN=sb(f"DEN{i}", [128, 1], F32), RD=sb(f"RD{i}", [128, 1], F32)))
        PB = [es.enter_context(nc.psum_tensor(f"PB{i}", [128, 512], F32)) for i in range(8)]

        engsem = {e: es.enter_context(nc.semaphore(f"s_{e}")) for e in Prog.ENG}
        dma_names = ([f"cast{i}" for i in range(NCAST)] + [f"ring{i}" for i in range(NSLOT)]
                     + [f"xl{i}" for i in range(NCH)] + ["yo0", "yo1", "cst", "lnp", "mem"])
        dmasem = {n: es.enter_context(nc.semaphore(f"d_{n}")) for n in dma_names}

        def bc(t, off, n):
            return bass.AP(t, off, [[0, 128], [1, n]])

        P.add("pool", lambda eng: eng.iota(IOT[:, :], [[1, 128]], base=0, channel_multiplier=-1),
              writes=["IOT"])
        P.add("dve", lambda eng: eng.tensor_scalar(IDF[:, :], IOT[:, :], 0.0, None, op0=ALU.is_equal),
              reads=["IOT"], writes=["IDF"])
        P.add("dve", lambda eng: eng.memset(ONESB[:, :], 1.0), writes=["ONESB"])
        ONESF = IOT[:, :].bitcast(F32)
        P.add("dve", lambda eng: eng.memset(ONESF, 1.0), reads=["IDF"], writes=["IOT"])
        P.add("dve", lambda eng: eng.memset(NEGH[:, :], -0.5), writes=["NEGH"])
        for i in range(2):
            P.add("dve", lambda eng, i=i: eng.memset(RT[i]["SELP"][:, :, :], -1e30), writes=[("SELP", i)])
        P.add("act", lambda eng: eng.dma_start(out=RW[:, :, :],
                                               in_=router_w_t.ap().rearrange("(k p) n -> p k n", p=128)),
              writes=["RW"], dma="cst")
        P.add("act", lambda eng: eng.dma_start(out=RB[:, :], in_=bc(router_b_t, 0, NE)),
              writes=["RB"], dma="cst")
        for l in layers:
            for (dstt, srct, nm) in ((G1T[l], ln1_g_t, "G1T"), (B1T[l], ln1_b_t, "B1T")):
                def col_dma(eng, dstt=dstt, srct=srct, l=l):
                    with nc.allow_non_contiguous_dma(reason="tiny per-partition LN column load"):
                        return eng.dma_start(out=dstt[:, :], in_=srct.ap()[l].rearrange("(k p) -> p k", p=128))
                P.add("act", col_dma, writes=[(nm, l)], dma="cst")
        if 0 in layers:
            P.add("act", lambda eng: eng.dma_start(out=SGUG[:, :], in_=bc(sgu_ln_g_t, 0, 512)),
                  writes=["SGUG"], dma="cst")
            P.add("act", lambda eng: eng.dma_start(out=SGUB[:, :], in_=bc(sgu_ln_b_t, 0, 512)),
                  writes=["SGUB"], dma="cst")
            P.add("act", lambda eng: eng.dma_start(out=BSB[:, :], in_=bc(sgu_b_t, 0, 512)),
                  writes=["BSB"], dma="cst")
            P.add("act", lambda eng: eng.dma_start(
                out=WSF[:, :, :], in_=sgu_w_t.ap()[0].rearrange("h i j -> i h j")),
                writes=["WSF", "Yc"], dma="cst")
            for h in range(4):
                P.add("pe", lambda eng, h=h: eng.transpose(PB[0][:, h * 128:(h + 1) * 128],
                                                           WSF[:, h, :], IDF[:, :]),
                      reads=["WSF", "Yc", "IDF"], writes=[("PB", 0)])
            P.add("dve", lambda eng: eng.tensor_copy(
                WST[:, :, :], PB[0][:, :].rearrange("p (h i) -> p h i", h=4)),
                reads=[("PB", 0)], writes=["WST"])
            P.add("dve", lambda eng: eng.memset(WST[64:128, :, 0:64], 0.0), writes=["WST"])
        if 1 in layers:
            for jj in range(3):
                def cw_dma(eng, jj=jj):
                    with nc.allow_non_contiguous_dma(reason="tiny conv weight transpose"):
                        return eng.dma_start(out=CWT[:, jj, :],
                                             in_=conv_w_t.ap()[0, jj].rearrange("(fc p) -> p fc", p=128))
                P.add("act", cw_dma, writes=["CWT"], dma="cst")

        stream = []
        for sq in range(n_seq):
            for tq in range(tiles_per_seq):
                if tq == 0:
                    for l in layers:
                        stream += [("kv", l, 0), ("kv", l, 1)]
                for l in layers:
                    nin = 3 if l == 0 else 4
                    stream += [("in", l, j) for j in range(nin)]
                    stream += [("out", l, j) for j in range(2)]
                    for e in range(NE):
                        stream += [("g", l, e), ("u", l, e), ("d", l, e)]
        ring = {"next": 0, "released": [False] * len(stream), "cur": 0}

        cast_state = {"done": set(), "ptr": 0, "n": 0}
        CAST_AHEAD = 12

        def cast_upto(i_max):
            while cast_state["ptr"] <= min(i_max, len(stream) - 1):
                key = stream[cast_state["ptr"]]
                cast_state["ptr"] += 1
                u = unit_ids[key]
                if u in cast_state["done"]:
                    continue
                cast_state["done"].add(u)
                src = unit_src[key]
                if key[0] == "d":
                    dst = wbf[u].rearrange("p (mc n) -> p mc n", mc=4)
                else:
                    dst = wbf[u].rearrange("p (kc n) -> p kc n", kc=8)
                P.add("pool", lambda eng, dst=dst, src=src: eng.dma_start(out=dst, in_=src),
                      writes=[("wbf", u)], dma=f"cast{cast_state['n'] % NCAST}")
                cast_state["n"] += 1

        def ring_pump():
            while ring["next"] < len(stream):
                i = ring["next"]
                if i >= NSLOT and not ring["released"][i - NSLOT]:
                    break
                cast_upto(i + CAST_AHEAD)
                s = i % NSLOT
                u = unit_ids[stream[i]]
                P.add("sp", lambda eng, s=s, u=u: eng.dma_start(out=RING[:, s, :], in_=wbf[u]),
                      reads=[("wbf", u)], writes=[("ring", s)], dma=f"ring{s}")
                ring["next"] += 1

        def ring_take(key):
            i = ring["cur"]
            assert stream[i] == key, (stream[i], key)
            ring["cur"] += 1
            if ring["next"] <= i:
                ring_pump()
            assert ring["next"] > i, "ring deadlock"
            return i % NSLOT, i

        def ring_release(i):
            ring["released"][i] = True
            ring_pump()

        def ring_kn(s):
            return RING[:, s, :].rearrange("p (kc n) -> p kc n", kc=8)

        def ring_d(s):
            return RING[:, s, :].rearrange("p (mc n) -> p mc n", mc=4)

        ring_pump()

        cnt = {"ev": 0, "yb": 0, "vg": 0, "u": 0, "pb": 0}

        def next_bank():
            cnt["pb"] += 1
            return cnt["pb"] % 3

        def seq(g):
            for _ in g:
                pass

        def par(*gens):
            act_ = list(gens)
            while act_:
                for g in list(act_):
                    try:
                        next(g)
                    except StopIteration:
                        act_.remove(g)

        def run_tasks(tasks):
            done, active, pending = set(), {}, dict(tasks)

            def start_ready():
                for n in list(pending):
                    g, deps = pending[n]
                    if deps <= done:
                        active[n] = g
                        del pending[n]
            start_ready()
            while active:
                for n in list(active):
                    try:
                        next(active[n])
                    except StopIteration:
                        del active[n]
                        done.add(n)
                        start_ready()
            assert not pending

        def g_tasks(tasks):
            done, active, pending = set(), {}, dict(tasks)

            def start_ready():
                for n in list(pending):
                    g, deps = pending[n]
                    if deps <= done:
                        active[n] = g
                        del pending[n]
            start_ready()
            while active:
                for n in list(active):
                    try:
                        next(active[n])
                    except StopIteration:
                        del active[n]
                        done.add(n)
                        start_ready()
                yield
            assert not pending

        def g_par(*gens):
            act_ = list(gens)
            while act_:
                for g in list(act_):
                    try:
                        next(g)
                    except StopIteration:
                        act_.remove(g)
                yield

        def chain(*gens):
            for g in gens:
                yield from g

        def evac(out, in_, reads, writes):
            cnt["ev"] += 1
            if cnt["ev"] % 2:
                P.add("act", lambda eng: eng.copy(out, in_), reads=reads, writes=writes)
            else:
                P.add("dve", lambda eng: eng.tensor_copy(out, in_), reads=reads, writes=writes)

        def xt_keys(c):
            return [("XT", c, 0), ("XT", c, 1)]

        def g_transposes_to_xt(src, src_key, c, also_f32, banks):
            for kq in range(2):
                bank = banks[kq]
                for j in range(4):
                    k = kq * 4 + j
                    P.add("pe", lambda eng, bank=bank, j=j, k=k: eng.transpose(
                        PB[bank][:, j * 128:(j + 1) * 128], src[:, k * 128:(k + 1) * 128], IDF[:, :]),
                        reads=[src_key, "IDF"], writes=[("PB", bank)])
                yield
                pv = PB[bank][:, :].rearrange("p (j t) -> p j t", j=4)
                P.add("act", lambda eng, pv=pv, kq=kq: eng.copy(
                    XT[:, kq * 4:(kq + 1) * 4, c * 128:(c + 1) * 128], pv),
                    reads=[("PB", bank)], writes=[("XT", c, kq)])
                assert not also_f32
                yield

        def rstd_from_mv(s):
            P.add("pool", lambda eng: eng.tensor_scalar(VEs[s][:, :], MVs[s][:, 1:2], EPS, None, op0=ALU.add),
                  reads=[("MV", s)], writes=[("VE", s)])
            P.add("pool", lambda eng: eng.tensor_tensor(RSTDs[s][:, :], VEs[s][:, :], NEGH[:, :], op=ALU.pow),
                  reads=[("VE", s), "NEGH"], writes=[("RSTD", s)])

        def g_layer_norm_rows(src, dst, src_key, dst_key, gt, bt, gkey, bkey, s):
            for hf in range(2):
                P.add("dve", lambda eng, hf=hf: eng.bn_stats(STs[s][:, hf, :], src[:, hf * 512:(hf + 1) * 512]),
                      reads=[src_key], writes=[("ST", s)])
            P.add("dve", lambda eng: eng.bn_aggr(MVs[s][:, :], STs[s][:, :, :].rearrange("p a b -> p (a b)")),
                  reads=[("ST", s)], writes=[("MV", s)])
            yield
            rstd_from_mv(s)
            yield
            P.add("dve", lambda eng: eng.scalar_tensor_tensor(
                dst, src, MVs[s][:, 0:1], gt[:, :], op0=ALU.subtract, op1=ALU.mult),
                reads=[src_key, ("MV", s), gkey], writes=[dst_key])
            yield
            P.add("dve", lambda eng: eng.scalar_tensor_tensor(
                dst, dst, RSTDs[s][:, 0:1], bt[:, :], op0=ALU.mult, op1=ALU.add),
                reads=[("RSTD", s), bkey], writes=[dst_key])
            yield

        def g_loadx(row0, c, lane):
            P.add("act", lambda eng: eng.dma_start(
                out=R[:, c, :], in_=x_d[row0 + c * 128: row0 + (c + 1) * 128, :]),
                writes=[("R", c)], dma=f"xl{c}")
            yield
            yield from g_transposes_to_xt(R[:, c, :], ("R", c), c, False, [6 + lane, 6 + lane])

        def compute_kv(sq):
            for mc in range(2):
                yb = Yb[mc]
                P.add("act", lambda eng, mc=mc, yb=yb: eng.dma_start(
                    out=yb[:, :], in_=mem_d[sq * N_MEM + mc * 128: sq * N_MEM + (mc + 1) * 128, :]),
                    writes=[("Y", mc)], dma="mem")
                for kq in range(2):
                    bank = 6 + kq
                    for j in range(4):
                        k = kq * 4 + j
                        P.add("pe", lambda eng, bank=bank, j=j, k=k, yb=yb: eng.transpose(
                            PB[bank][:, j * 128:(j + 1) * 128], yb[:, k * 128:(k + 1) * 128], IDF[:, :]),
                            reads=[("Y", mc), "IDF"], writes=[("PB", bank)])
                    pv = PB[bank][:, :].rearrange("p (j t) -> p j t", j=4)
                    evac(MT[:, kq * 4:(kq + 1) * 4, mc * 128:(mc + 1) * 128], pv,
                         [("PB", bank)], [("MT", mc, kq)] + [("ET", a_, b_) for a_ in range(2) for b_ in range(2)])
            mt_keys = [("MT", mc, kq) for mc in range(2) for kq in range(2)]
            for l in layers:
                sk, ik = ring_take(("kv", l, 0))
                sv, iv = ring_take(("kv", l, 1))
                wk, wv = ring_kn(sk), ring_kn(sv)
                for h in range(4):
                    bank = h % 2
                    for k in range(8):
                        P.add("pe", lambda eng, bank=bank, h=h, k=k, wk=wk: eng.matmul(
                            PB[bank][:, 0:N_MEM], lhsT=wk[:, k, h * 128:(h + 1) * 128], rhs=MT[:, k, :],
                            start=(k == 0), stop=(k == 7)),
                            reads=[("ring", sk)] + mt_keys, writes=[("PB", bank)])
                    evac(KT[l][:, h, :], PB[bank][:, 0:N_MEM], [("PB", bank)], [("KT", l)])
                for mc in range(2):
                    bank = mc % 2
                    for k in range(8):
                        P.add("pe", lambda eng, bank=bank, mc=mc, k=k, wv=wv: eng.matmul(
                            PB[bank][:, :], lhsT=MT[:, k, mc * 128:(mc + 1) * 128], rhs=wv[:, k, :],
                            start=(k == 0), stop=(k == 7)),
                            reads=[("ring", sv)] + mt_keys, writes=[("PB", bank)])
                    evac(Vv[l][:, mc, :], PB[bank][:, :], [("PB", bank)], [("V", l)])
                ring_release(ik)
                ring_release(iv)

        def load_ln1_params(l):
            srcs = [ln1_g_t, ln1_b_t]
            for i in range(2):
                P.add("act", lambda eng, i=i: eng.dma_start(out=LNP[i][:, :], in_=bc(srcs[i], l * D, D)),
                      writes=[("LNP", i)], dma="lnp")
                P.add("act", lambda eng, i=i: eng.mul(LNP[i][:, :], LNP[i][:, :], ALPHA), writes=[("LNP", i)])

        def load_ln2_params(l):
            srcs = [ln2_g_t, ln2_b_t]
            for i in range(2):
                P.add("act", lambda eng, i=i: eng.dma_start(out=LNP[2 + i][:, :], in_=bc(srcs[i], l * D, D)),
                      writes=[("LNP", 2 + i)], dma="lnp")

        def g_att_blk(l, cp):
            for h in range(4):
                banks = (4, 5)
                hs = 0
                for mc in range(2):
                    P.add("pe", lambda eng, h=h, mc=mc, banks=banks: eng.matmul(
                        PB[banks[mc]][:, :], lhsT=KT[l][:, h, mc * 128:(mc + 1) * 128], rhs=QT[:, h, :],
                        start=True, stop=True),
                        reads=[("QT", h), ("KT", l)], writes=[("PB", banks[mc])])
                yield
                for mc in range(2):
                    P.add("act", lambda eng, mc=mc, banks=banks, hs=hs: eng.activation(
                        ETall[:, hs, mc, :], PB[banks[mc]][:, :], AF.Exp, scale=SCALE),
                        reads=[("PB", banks[mc])], writes=[("ET", hs, mc)])
                yield
                for mc in range(2):
                    P.add("pe", lambda eng, h=h, mc=mc, banks=banks, hs=hs: eng.matmul(
                        PB[banks[0]][:, :], lhsT=Vv[l][:, mc, h * 128:(h + 1) * 128], rhs=ETall[:, hs, mc, :],
                        start=(mc == 0), stop=(mc == 1)),
                        reads=[("V", l), ("ET", hs, mc)], writes=[("PB", banks[0])])
                for c4 in range(4):
                    for mc in range(2):
                        P.add("pe", lambda eng, c4=c4, mc=mc, banks=banks, hs=hs: eng.matmul(
                            PB[banks[1]][:, c4:c4 + 1], lhsT=ETall[:, hs, mc, c4 * 128:(c4 + 1) * 128],
                            rhs=ONESB[:, 0:1], start=(mc == 0), stop=(mc == 1)),
                            reads=["ONESB", ("ET", hs, mc)], writes=[("PB", banks[1])])
                yield
                P.add("act", lambda eng, banks=banks: eng.copy(PVS, PB[banks[0]][:, :]),
                      reads=[("PB", banks[0])], writes=PVS_KEYS)
                P.add("dve", lambda eng, banks=banks: eng.reciprocal(RT4[:, :], PB[banks[1]][:, 0:4]),
                      reads=[("PB", banks[1])], writes=["RT4"])
                for c4 in range(4):
                    P.add("dve", lambda eng, c4=c4: eng.tensor_scalar(
                        DG[:, c4, :], IDF[:, :], RT4[:, c4:c4 + 1], None, op0=ALU.mult),
                        reads=["IDF", "RT4"], writes=["RDEN"])
                yield
                for c4 in range(4):
                    P.add("pe", lambda eng, c4=c4, banks=banks: eng.matmul(
                        PB[banks[1]][:, c4 * 128:(c4 + 1) * 128], lhsT=ONESF, rhs=DG[:, c4, :],
                        start=True, stop=True),
                        reads=["IOT", "RDEN"], writes=[("PB", banks[1])])
                yield
                P.add("dve", lambda eng, h=h, banks=banks: eng.tensor_tensor(
                    CATTs[cp][:, 4 + h, :], PVS, PB[banks[1]][:, :], op=ALU.mult),
                    reads=PVS_KEYS + [("PB", banks[1])], writes=[("CATT", cp, 4 + h, j) for j in range(4)])
                yield

        def q_proj(blk, wq, sq_slot):
            bs = slice(blk * 512, (blk + 1) * 512)
            xk = [k for c in range(blk * 4, blk * 4 + 4) for k in xt_keys(c)]
            for h in range(4):
                bank = h
                for k in range(8):
                    P.add("pe", lambda eng, bank=bank, h=h, k=k: eng.matmul(
                        PB[bank][:, :], lhsT=wq[:, k, h * 128:(h + 1) * 128], rhs=XT[:, k, bs],
                        start=(k == 0), stop=(k == 7)),
                        reads=[("ring", sq_slot)] + xk, writes=[("PB", bank)])
                evac(QT[:, h, :], PB[bank][:, :], [("PB", bank)], [("QT", h)])
                yield

        def g_out_a(c, j, wo, so, lane, cp):
            Y, ykey, bank = Yb[lane], ("Y", lane), 6 + lane
            cs = slice(j * 128, (j + 1) * 128)
            catt_keys = [("CATT", cp, fc, j) for fc in range(8)]
            for hf in range(2):
                for fc in range(8):
                    P.add("pe", lambda eng, fc=fc, hf=hf: eng.matmul(
                        PB[bank][:, :], lhsT=CATTs[cp][:, fc, cs], rhs=wo[hf][:, fc, :],
                        start=(fc == 0), stop=(fc == 7)),
                        reads=catt_keys + [("ring", so[hf])], writes=[("PB", bank)])
                yield
                P.add("dve", lambda eng, hf=hf: eng.scalar_tensor_tensor(
                    Y[:, hf * 512:(hf + 1) * 512], R[:, c, hf * 512:(hf + 1) * 512], ALPHA, PB[bank][:, :],
                    op0=ALU.mult, op1=ALU.add),
                    reads=[("R", c), ("PB", bank)], writes=[ykey])
                yield

        def g_out_b(l, c, lane):
            Y, ykey, bank = Yb[lane], ("Y", lane), 6 + lane
            X1T, NMR, T = X1Ts[lane], NMRs[lane], RT[lane]
            s0 = lane
            tk = lambda n: (n, lane)
            for hf in range(2):
                P.add("dve", lambda eng, hf=hf: eng.bn_stats(STs[s0][:, hf, :], Y[:, hf * 512:(hf + 1) * 512]),
                      reads=[ykey], writes=[("ST", s0)])
            P.add("dve", lambda eng: eng.bn_aggr(MVs[s0][:, :], STs[s0][:, :, :].rearrange("p a b -> p (a b)")),
                  reads=[("ST", s0)], writes=[("MV", s0)])
            yield
            rstd_from_mv(s0)
            yield
            P.add("dve", lambda eng: eng.tensor_scalar(NMR[:, :], MVs[s0][:, 0:1], RSTDs[s0][:, 0:1], -1.0,
                                                       op0=ALU.mult, op1=ALU.mult),
                  reads=[("MV", s0), ("RSTD", s0)], writes=[tk("NMR")])
            yield
            P.add("act", lambda eng: eng.activation(Y[:, :], Y[:, :], AF.Identity,
                                                    bias=NMR[:, 0:1], scale=RSTDs[s0][:, 0:1]),
                  reads=[tk("NMR"), ("RSTD", s0)], writes=[ykey])
            yield
            for kq in range(2):
                for j in range(4):
                    k = kq * 4 + j
                    P.add("pe", lambda eng, j=j, k=k: eng.transpose(
                        PB[bank][:, j * 128:(j + 1) * 128], Y[:, k * 128:(k + 1) * 128], IDF[:, :]),
                        reads=[ykey, "IDF"], writes=[("PB", bank)])
                yield
                for j in range(4):
                    k = kq * 4 + j
                    P.add("act", lambda eng, j=j, k=k: eng.activation(
                        X1T[:, k, :], PB[bank][:, j * 128:(j + 1) * 128], AF.Identity,
                        bias=B1T[l][:, k:k + 1], scale=G1T[l][:, k:k + 1]),
                        reads=[("PB", bank), ("G1T", l), ("B1T", l)], writes=[("X1T", lane, kq)])
                yield
                P.add("act", lambda eng, kq=kq: eng.copy(
                    XT[:, kq * 4:(kq + 1) * 4, c * 128:(c + 1) * 128], X1T[:, kq * 4:(kq + 1) * 4, :]),
                    reads=[("X1T", lane, kq)], writes=[("XT", c, kq)])
                yield
            for k in range(8):
                P.add("pe", lambda eng, k=k: eng.matmul(
                    PB[bank][:, 0:NE], lhsT=X1T[:, k, :], rhs=RW[:, k, :], start=(k == 0), stop=(k == 7)),
                    reads=[("X1T", lane, 0), ("X1T", lane, 1), "RW"], writes=[("PB", bank)])
            yield
            EX, SC, SEL, SELP, M8, GS = T["EX"], T["SC"], T["SEL"], T["SELP"], T["M8"], T["GS"]
            GMX, GM, MSK, GT, DEN, RD = T["GMX"], T["GM"], T["MSK"], T["GT"], T["DEN"], T["RD"]
            P.add("act", lambda eng: eng.activation(EX[:, :], PB[bank][:, 0:NE], AF.Exp, scale=-1.0),
                  reads=[("PB", bank)], writes=[tk("EX")])
            yield
            P.add("dve", lambda eng: eng.tensor_scalar(SC[:, :], EX[:, :], 1.0, None, op0=ALU.add),
                  reads=[tk("EX")], writes=[tk("SC")])
            P.add("dve", lambda eng: eng.reciprocal(SC[:, :], SC[:, :]), writes=[tk("SC")])
            P.add("dve", lambda eng: eng.tensor_tensor(SEL[:, :], SC[:, :], RB[:, :], op=ALU.add),
                  reads=[tk("SC"), "RB"], writes=[tk("SEL")])
            yield
            sel3 = SEL[:, :].rearrange("p (g k) -> p g k", g=4)
            P.add("dve", lambda eng: eng.tensor_copy(SELP[:, :, 0:4], sel3), reads=[tk("SEL")], writes=[tk("SELP")])
            for g in range(4):
                P.add("dve", lambda eng, g=g: eng.max(M8[:, g, :], SELP[:, g, :]),
                      reads=[tk("SELP")], writes=[tk("M8")])
            yield
            P.add("dve", lambda eng: eng.tensor_tensor(GS[:, :], M8[:, :, 0], M8[:, :, 1], op=ALU.add),
                  reads=[tk("M8")], writes=[tk("GS")])
            P.add("dve", lambda eng: eng.tensor_reduce(GMX[:, :], GS[:, :], axis=AX.X, op=ALU.max),
                  reads=[tk("GS")], writes=[tk("GMX")])
            P.add("dve", lambda eng: eng.tensor_scalar(GM[:, :], GS[:, :], GMX[:, 0:1], None, op0=ALU.is_ge),
                  reads=[tk("GS"), tk("GMX")], writes=[tk("GM")])
            yield
            msk3 = MSK[:, :].rearrange("p (g k) -> p g k", g=4)
            P.add("dve", lambda eng: eng.tensor_tensor(
                msk3, sel3, M8[:, :, 1:2].to_broadcast([128, 4, 4]), op=ALU.is_ge),
                reads=[tk("SEL"), tk("M8")], writes=[tk("MSK")])
            P.add("dve", lambda eng: eng.tensor_tensor(
                msk3, msk3, GM[:, :].unsqueeze(2).to_broadcast([128, 4, 4]), op=ALU.mult),
                reads=[tk("GM")], writes=[tk("MSK")])
            P.add("dve", lambda eng: eng.tensor_tensor(GT[:, :], MSK[:, :], SC[:, :], op=ALU.mult),
                  reads=[tk("MSK"), tk("SC")], writes=[tk("GT")])
            yield
            P.add("dve", lambda eng: eng.tensor_reduce(DEN[:, :], GT[:, :], axis=AX.X, op=ALU.add),
                  reads=[tk("GT")], writes=[tk("DEN")])
            P.add("dve", lambda eng: eng.reciprocal(RD[:, :], DEN[:, :]), reads=[tk("DEN")], writes=[tk("RD")])
            P.add("dve", lambda eng: eng.tensor_scalar(CW[:, c, :], GT[:, :], RD[:, 0:1], None, op0=ALU.mult),
                  reads=[tk("GT"), tk("RD")], writes=[("CW", c)])
            yield
            P.add("dve", lambda eng: eng.tensor_tensor(R[:, c, :], Y[:, :], LNP[0][:, :], op=ALU.mult),
                  reads=[ykey, ("LNP", 0)], writes=[("R", c)])
            yield
            P.add("dve", lambda eng: eng.tensor_tensor(R[:, c, :], R[:, c, :], LNP[1][:, :], op=ALU.add),
                  reads=[("LNP", 1)], writes=[("R", c)])
            yield

        def g_phase1_a(l, blk, W, cp, do_q=True):
            wu, wv, wq, su, sv_, sq_ = W
            bs = slice(blk * 512, (blk + 1) * 512)
            xk = [k for c in range(blk * 4, blk * 4 + 4) for k in xt_keys(c)]
            for j in range(4):
                c = blk * 4 + j
                bank = next_bank()
                vg_i = cnt["vg"] % 2
                cnt["vg"] += 1
                vg = VG[vg_i]
                s = 2 + vg_i
                for k in range(8):
                    P.add("pe", lambda eng, bank=bank, k=k, c=c: eng.matmul(
                        PB[bank][:, :], lhsT=XT[:, k, c * 128:(c + 1) * 128], rhs=wv[:, k, :],
                        start=(k == 0), stop=(k == 7)),
                        reads=xt_keys(c) + [("ring", sv_)], writes=[("PB", bank)])
                P.add("act", lambda eng, bank=bank, vg=vg: eng.activation(vg[:, :], PB[bank][:, :], AF.Gelu),
                      reads=[("PB", bank)], writes=[("VG", vg_i)])
                yield
                P.add("dve", lambda eng, vg=vg, s=s: eng.bn_stats(STs[s][:, 0, :], vg[:, :]),
                      reads=[("VG", vg_i)], writes=[("ST", s)])
                P.add("dve", lambda eng, s=s: eng.bn_aggr(MVs[s][:, :], STs[s][:, 0, :]),
                      reads=[("ST", s)], writes=[("MV", s)])
                rstd_from_mv(s)
                yield
                P.add("dve", lambda eng, vg=vg, s=s: eng.scalar_tensor_tensor(
                    vg[:, :], vg[:, :], MVs[s][:, 0:1], SGUG[:, :], op0=ALU.subtract, op1=ALU.mult),
                    reads=[("MV", s), "SGUG"], writes=[("VG", vg_i)])
                P.add("dve", lambda eng, vg=vg, j=j, s=s: eng.scalar_tensor_tensor(
                    VN[:, j, :], vg[:, :], RSTDs[s][:, 0:1], SGUB[:, :], op0=ALU.mult, op1=ALU.add),
                    reads=[("VG", vg_i), ("RSTD", s), "SGUB"], writes=[("VN", j)])
                yield
            if do_q:
                yield from q_proj(blk, wq, sq_)
            for h in range(4):
                bank = next_bank()
                u_i = cnt["u"] % 2
                cnt["u"] += 1
                U = Ub[u_i]
                for k in range(8):
                    P.add("pe", lambda eng, bank=bank, h=h, k=k: eng.matmul(
                        PB[bank][:, :], lhsT=wu[:, k, h * 128:(h + 1) * 128], rhs=XT[:, k, bs],
                        start=(k == 0), stop=(k == 7)),
                        reads=[("ring", su)] + xk, writes=[("PB", bank)])
                P.add("act", lambda eng, bank=bank, U=U: eng.activation(U[:, :], PB[bank][:, :], AF.Gelu),
                      reads=[("PB", bank)], writes=[("U", u_i)])
                yield
                for j in range(4):
                    P.add("pe", lambda eng, h=h, j=j: eng.matmul(
                        PB[3][:, j * 128:(j + 1) * 128], lhsT=VN[:, j, h * 128:(h + 1) * 128],
                        rhs=WST[:, h, :], start=True, stop=True),
                        reads=[("VN", j), "WST"], writes=[("PB", 3)])
                yield
                P.add("dve", lambda eng, h=h: eng.tensor_tensor(
                    Tt[:, :].rearrange("p (j i) -> p j i", j=4),
                    PB[3][:, :].rearrange("p (j i) -> p j i", j=4),
                    BSB[:, h * 128:(h + 1) * 128].unsqueeze(1).to_broadcast([128, 4, 128]), op=ALU.add),
                    reads=[("PB", 3), "BSB"], writes=["Tt"])
                P.add("dve", lambda eng, h=h, U=U: eng.tensor_tensor(
                    CATTs[cp][:, h, :], Tt[:, :], U[:, :], op=ALU.mult),
                    reads=["Tt", ("U", u_i)], writes=[("CATT", cp, h, j) for j in range(4)])
                yield

        def g_phase1_b(l, blk, W, cp, do_q=True):
            wb, wc, wx, wq, sb_, sc_, sx_, sq_ = W
            bs = slice(blk * 512, (blk + 1) * 512)
            xk = [k for c in range(blk * 4, blk * 4 + 4) for k in xt_keys(c)]
            if do_q:
                yield from q_proj(blk, wq, sq_)
            for fc in range(4):
                fs = slice(fc * 128, (fc + 1) * 128)
                bb, bcg, bx = (0, 1, 2) if fc % 2 == 0 else (3, 1, 2)
                for (w, sl, bank) in ((wb, sb_, bb), (wc, sc_, bcg), (wx, sx_, bx)):
                    for k in range(8):
                        P.add("pe", lambda eng, w=w, bank=bank, k=k, fs=fs: eng.matmul(
                            PB[bank][:, :], lhsT=w[:, k, fs], rhs=XT[:, k, bs],
                            start=(k == 0), stop=(k == 7)),
                            reads=[("ring", sl)] + xk, writes=[("PB", bank)])
                    yield
                P.add("act", lambda eng, bcg=bcg: eng.copy(CGs[:, :], PB[bcg][:, :]), reads=[("PB", bcg)], writes=["Tt"])
                P.add("act", lambda eng, fc=fc: eng.copy(HTw[:, 0:2], HS[:, fc, :]),
                      reads=[("HS", fc)], writes=["HTw"])
                yield
                P.add("dve", lambda eng, bx=bx: eng.tensor_tensor(
                    HTw[:, 2:514], CGs[:, :], PB[bx][:, :], op=ALU.mult),
                    reads=["Tt", ("PB", bx)], writes=["HTw"])
                P.add("act", lambda eng, fc=fc: eng.activation(
                    Yc[:, :], HTw[:, 2:514], AF.Identity, scale=CWT[:, 2, fc:fc + 1]),
                    reads=["HTw", "CWT"], writes=["Yc"])
                yield
                P.add("dve", lambda eng, fc=fc: eng.scalar_tensor_tensor(
                    Yc[:, :], HTw[:, 1:513], CWT[:, 1, fc:fc + 1], Yc[:, :], op0=ALU.mult, op1=ALU.add),
                    reads=["HTw", "CWT"], writes=["Yc"])
                P.add("dve", lambda eng, fc=fc: eng.scalar_tensor_tensor(
                    Yc[:, :], HTw[:, 0:512], CWT[:, 0, fc:fc + 1], Yc[:, :], op0=ALU.mult, op1=ALU.add),
                    reads=["HTw", "CWT"], writes=["Yc"])
                yield
                P.add("dve", lambda eng, fc=fc, bb=bb: eng.tensor_tensor(
                    CATTs[cp][:, fc, :], Yc[:, :], PB[bb][:, :], op=ALU.mult),
                    reads=["Yc", ("PB", bb)], writes=[("CATT", cp, fc, j) for j in range(4)])
                P.add("act", lambda eng, fc=fc: eng.copy(HS[:, fc, :], HTw[:, 512:514]),
                      reads=["HTw"], writes=[("HS", fc)])
                yield

        def mixer(l, first_tile_of_seq, pre_gens):
            nin = 3 if l == 0 else 4
            slots = [ring_take(("in", l, j)) for j in range(nin)]
            oslots = [ring_take(("out", l, j)) for j in range(2)]
            W = tuple(ring_kn(s[0]) for s in slots) + tuple(s[0] for s in slots)
            wo = [ring_kn(oslots[0][0]), ring_kn(oslots[1][0])]
            so = [oslots[0][0], oslots[1][0]]
            ph1 = g_phase1_a if l == 0 else g_phase1_b
            if l == 1 and first_tile_of_seq:
                P.add("dve", lambda eng: eng.memset(HS[:, :, :], 0.0), writes=[("HS", fc) for fc in range(4)])
            def stream_a(blk):
                wq_, sq_ = (W[2], W[5]) if l == 0 else (W[3], W[7])
                return chain(q_proj(blk, wq_, sq_),
                             g_par(g_att_blk(l, blk % 2), ph1(l, blk, W, blk % 2, do_q=False)))

            def stream_b(blk):
                tasks = {}
                for j in range(4):
                    c = blk * 4 + j
                    deps = {f"O{j - 2}"} if j >= 2 else set()
                    tasks[f"O{j}"] = (chain(g_out_a(c, j, wo, so, j % 2, blk % 2), g_out_b(l, c, j % 2)), deps)
                return g_tasks(tasks)

            par(*pre_gens, stream_a(0))
            for blk in range(1, NBLK):
                par(stream_a(blk), stream_b(blk - 1))
            for s in slots:
                ring_release(s[1])

            def release_out():
                for s in oslots:
                    ring_release(s[1])
            return stream_b(NBLK - 1), release_out

        def moe(l, tail, release_out):
            assert NBLK == 2
            items = [(0, 0), (1, 0), (0, 1), (1, 1)] + [(e, blk) for e in range(2, NE) for blk in range(NBLK)]
            last_item = {}
            for i, (e, _) in enumerate(items):
                last_item[e] = i
            order = [(k, e) for e in range(NE) for k in ("g", "u", "d")]
            taken = {}
            st = {"ptr": 0}

            def need(kind, e):
                tgt = order.index((kind, e))
                while st["ptr"] <= tgt:
                    k2, e2 = order[st["ptr"]]
                    taken[(k2, e2)] = ring_take((k2, l, e2))
                    st["ptr"] += 1
                return taken[(kind, e)]

            def g_p1(idx):
                e, blk = items[idx]
                su, _ = need("u", e)
                sg, _ = taken[("g", e)]
                wg, wu = ring_kn(sg), ring_kn(su)
                bs = slice(blk * 512, (blk + 1) * 512)
                xk = [k for c in range(blk * 4, blk * 4 + 4) for k in xt_keys(c)]
                hT = HTb[idx % 2]
                for m in range(4):
                    ms = slice(m * 128, (m + 1) * 128)
                    bg, bu = m % 2, 2 + m % 2
                    for k in range(8):
                        P.add("pe", lambda eng, bg=bg, k=k, ms=ms: eng.matmul(
                            PB[bg][:, :], lhsT=wg[:, k, ms], rhs=XT[:, k, bs], start=(k == 0), stop=(k == 7)),
                            reads=[("ring", sg)] + xk, writes=[("PB", bg)])
                    yield
                    for k in range(8):
                        P.add("pe", lambda eng, bu=bu, k=k, ms=ms: eng.matmul(
                            PB[bu][:, :], lhsT=wu[:, k, ms], rhs=XT[:, k, bs], start=(k == 0), stop=(k == 7)),
                            reads=[("ring", su)] + xk, writes=[("PB", bu)])
                    sgt = SG[m % 2]
                    P.add("act", lambda eng, bg=bg, sgt=sgt: eng.activation(sgt[:, :], PB[bg][:, :], AF.Silu),
                          reads=[("PB", bg)], writes=[("SG", m % 2)])
                    P.add("dve", lambda eng, bu=bu, sgt=sgt, m=m: eng.tensor_tensor(
                        hT[:, m, :], sgt[:, :], PB[bu][:, :], op=ALU.mult),
                        reads=[("SG", m % 2), ("PB", bu)], writes=[("hT", idx % 2, m)])
                    yield

            def g_p2(idx, banks):
                e, blk = items[idx]
                sd, _ = need("d", e)
                wd = ring_d(sd)
                hT = HTb[idx % 2]
                hk = [("hT", idx % 2, m) for m in range(4)]
                nb = len(banks)
                for s in range(4):
                    c = blk * 4 + s
                    for hf in range(2):
                        bank = banks[(s * 2 + hf) % nb]
                        for m in range(4):
                            P.add("pe", lambda eng, bank=bank, m=m, s=s, hf=hf: eng.matmul(
                                PB[bank][:, :], lhsT=hT[:, m, s * 128:(s + 1) * 128],
                                rhs=wd[:, m, hf * 512:(hf + 1) * 512], start=(m == 0), stop=(m == 3)),
                                reads=hk + [("ring", sd)], writes=[("PB", bank)])
                        P.add("dve", lambda eng, bank=bank, c=c, hf=hf: eng.scalar_tensor_tensor(
                            R[:, c, hf * 512:(hf + 1) * 512], PB[bank][:, :], CW[:, c, e:e + 1],
                            R[:, c, hf * 512:(hf + 1) * 512], op0=ALU.mult, op1=ALU.add),
                            reads=[("PB", bank), ("CW", c)], writes=[("R", c)])
                        yield
                if idx == last_item[e]:
                    for k in ("g", "u", "d"):
                        ring_release(taken[(k, e)][1])

            par(tail, chain(g_p1(0), g_p1(1), g_p2(0, [4, 5])))
            release_out()
            load_ln2_params(l)
            n = len(items)
            for i in range(2, n - 1):
                seq(g_p1(i))
                seq(g_p2(i - 1, [4, 5, 6, 7]))
            seq(g_p2(n - 2, [4, 5, 6, 7]))
            return chain(g_p1(n - 1), g_p2(n - 1, [4, 5]))

        def g_ln2(c, lane, last, row0, next_row0):
            if last:
                Y = Yb[lane]
                yield from g_layer_norm_rows(R[:, c, :], Y[:, :], ("R", c), ("Y", lane), LNP[2], LNP[3],
                                             ("LNP", 2), ("LNP", 3), lane)
                P.add("act", lambda eng: eng.dma_start(
                    out=y_d[row0 + c * 128: row0 + (c + 1) * 128, :], in_=Y[:, :]),
                    reads=[("Y", lane)], dma=f"yo{lane}")
                yield
                if next_row0 is not None:
                    yield from g_loadx(next_row0, c, lane)
            else:
                yield from g_layer_norm_rows(R[:, c, :], R[:, c, :], ("R", c), ("R", c), LNP[2], LNP[3],
                                             ("LNP", 2), ("LNP", 3), lane)
                yield from g_transposes_to_xt(R[:, c, :], ("R", c), c, False, [6 + lane, 6 + lane])

        def ln2_stage(last, row0, next_row0, defer, moe_tail):
            def lanes(cs):
                return [chain(*[g_ln2(c, lane, last, row0, next_row0) for c in cs if c % 2 == lane])
                        for lane in range(2)]
            par(*lanes(range(0, 4)), moe_tail)
            if defer:
                return lanes(range(4, NCH))
            par(*lanes(range(4, NCH)))
            return []

        tiles = [(sq, tq) for sq in range(n_seq) for tq in range(tiles_per_seq)]
        par(*[chain(*[g_loadx(0, c, lane) for c in range(lane, NCH, 2)]) for lane in range(2)])
        pre = []
        for ti, (sq, tq) in enumerate(tiles):
            row0 = sq * seq_len + tq * TT
            next_row0 = None
            if ti + 1 < len(tiles):
                nsq, ntq = tiles[ti + 1]
                next_row0 = nsq * seq_len + ntq * TT
            if tq == 0:
                assert not pre
                compute_kv(sq)
            for li, l in enumerate(layers):
                last = li == len(layers) - 1
                load_ln1_params(l)
                tail, release_out = mixer(l, tq == 0, pre)
                moe_tail = moe(l, tail, release_out)
                if last:
                    defer = (ti + 1 < len(tiles)) and tiles[ti + 1][1] != 0
                else:
                    defer = True
                pre = ln2_stage(last, row0, next_row0, defer, moe_tail)

        P.emit(nc, engsem, dmasem)
    return nc


_WKEYS = ["w_in_a", "sgu_ln_g", "sgu_ln_b", "sgu_w", "sgu_b", "w_in_b", "conv_w", "w_kv", "w_out",
          "ln1_g", "ln1_b", "router_w", "router_b", "w_gate", "w_up", "w_down", "ln2_g", "ln2_b"]


def _in_map(x_c, mem_c, inputs):
    m = {"x": np.ascontiguousarray(x_c.reshape(-1, D), dtype=np.float32),
         "mem": np.ascontiguousarray(mem_c.reshape(-1, D), dtype=np.float32)}
    for k in _WKEYS:
        a = np.ascontiguousarray(np.asarray(inputs[k], dtype=np.float32))
        if k == "sgu_b":
            a = a.reshape(1, 512)
        if k == "router_b":
            a = a.reshape(1, NE)
        m[k] = a
    return m


def run(inputs, n_cores, layers=(0, 1), trace=False, dbg=None):
    x = np.asarray(inputs["x"], dtype=np.float32)
    mem = np.asarray(inputs["mem"], dtype=np.float32)
    B, S, _ = x.shape
    n_seq = B // n_cores
    nc = build(n_seq, S, list(layers), dbg=dbg)
    in_maps = [_in_map(x[i * n_seq:(i + 1) * n_seq], mem[i * n_seq:(i + 1) * n_seq], inputs)
               for i in range(n_cores)]
    res = run_bass_kernel_spmd(nc, in_maps, core_ids=list(range(n_cores)), trace=trace)
    out = np.concatenate([np.asarray(r["y"]).reshape(n_seq, S, D) for r in res.results], axis=0)
    return out.astype(np.float32), res


def kernel(**inputs):
    out, _ = run(inputs, 8)
    return out
```
